# Optimizing a Trainium2 kernel written in Bass

```python
import math
import jax, jax.numpy as jnp
from jax import lax
import numpy as np

D_MODEL = 2048
BATCH = 1
SEQ = 8192
DEPTH = 4

GRID_W = 64
CTX_LEN = 256
N_MIXERS = 4
ALPHA = (2.0 * DEPTH) ** 0.25
BETA = (8.0 * DEPTH) ** -0.25
Q_BLOCK = 128
ROPE_THETA = 10000.0
LN_EPS = 1e-6
RMS_EPS = 1e-6
N_MOD = 6

HEAD_DIM = 128
GQA_HEADS = D_MODEL // HEAD_DIM
GQA_KV_HEADS = GQA_HEADS // 4
GQA_GROUP = GQA_HEADS // GQA_KV_HEADS
GQA_QKV = (GQA_HEADS + 2 * GQA_KV_HEADS) * HEAD_DIM
DIFF_HEADS = D_MODEL // (2 * HEAD_DIM)

POOL_WINDOWS = (2, 4, 8, 16)
POOL_GROUP = D_MODEL // len(POOL_WINDOWS)

N_GROUPS = 4
EXPERTS_PER_GROUP = 4
N_EXPERTS = N_GROUPS * EXPERTS_PER_GROUP
TOP_K = 2
EXPERT_FF = 3 * D_MODEL // 8

kernel_name = "hybrid_interleaved_diffusion_trunk"


def _n_uses(mixer):
    return len(range(mixer, DEPTH, N_MIXERS))


def layer_norm(x, g, b):
    xf = x.astype(jnp.float32)
    mu = jnp.mean(xf, axis=-1, keepdims=True)
    var = jnp.mean(jnp.square(xf - mu), axis=-1, keepdims=True)
    return ((xf - mu) * lax.rsqrt(var + LN_EPS)).astype(x.dtype) * g + b


def rms_norm(x, g):
    xf = x.astype(jnp.float32)
    return (xf * lax.rsqrt(jnp.mean(xf * xf, axis=-1, keepdims=True) + RMS_EPS)).astype(x.dtype) * g


def modulate(t, shift, scale):
    return t * (1.0 + scale) + shift


def axial_rope_tables(rows, dtype):
    row = jnp.repeat(jnp.arange(rows, dtype=jnp.float32), GRID_W)
    col = jnp.tile(jnp.arange(GRID_W, dtype=jnp.float32), rows)
    axis_dim = HEAD_DIM // 2
    inv = ROPE_THETA ** (-jnp.arange(0, axis_dim, 2, dtype=jnp.float32) / axis_dim)
    ang = jnp.concatenate([row[:, None] * inv, col[:, None] * inv], axis=-1)
    return jnp.cos(ang).astype(dtype), jnp.sin(ang).astype(dtype)


def apply_axial_rope(x, cos, sin):
    qd = x.shape[-1] // 4
    xr1, xr2, xc1, xc2 = jnp.split(x, 4, axis=-1)
    cr, cc = cos[:, :qd], cos[:, qd:]
    sr, sc = sin[:, :qd], sin[:, qd:]
    return jnp.concatenate([xr1 * cr - xr2 * sr, xr2 * cr + xr1 * sr,
                            xc1 * cc - xc2 * sc, xc2 * cc + xc1 * sc], axis=-1)


def sweep_query_blocks(fn, q):
    n = q.shape[-2]
    qb = jnp.moveaxis(q.reshape(q.shape[:-2] + (n // Q_BLOCK, Q_BLOCK, q.shape[-1])), -3, 0)
    ob = jnp.moveaxis(lax.map(fn, qb), 0, -3)
    return ob.reshape(ob.shape[:-3] + (n, ob.shape[-1]))


def depthwise_conv3(u, w):
    return lax.conv_general_dilated(u, w[:, None, :], window_strides=(1,), padding=((1, 1),),
                                    dimension_numbers=('NWC', 'WIO', 'NWC'),
                                    feature_group_count=u.shape[-1])


def conv_mixer(h, hc, w_in, w_conv, w_out, need_ctx):
    def run(t):
        gate_b, gate_c, val = jnp.split(t @ w_in, 3, axis=-1)
        return (gate_b * depthwise_conv3(gate_c * val, w_conv)) @ w_out
    return run(h), (run(hc) if need_ctx else None)


def window_mean_minus_self(u, window):
    n = u.shape[1]
    cs = jnp.pad(jnp.cumsum(u.astype(jnp.float32), axis=1), ((0, 0), (1, 0), (0, 0)))
    t = jnp.arange(n)
    lo = jnp.clip(t - window // 2, 0, n)
    hi = jnp.clip(t + window - window // 2, 0, n)
    mean = (cs[:, hi] - cs[:, lo]) / (hi - lo).astype(jnp.float32)[None, :, None]
    return mean.astype(u.dtype) - u


def pool_mixer(h, hc, w_grp, scale, need_ctx):
    def run(t):
        parts = jnp.split(t, len(POOL_WINDOWS), axis=-1)
        pooled = jnp.stack([window_mean_minus_self(p, w) for p, w in zip(parts, POOL_WINDOWS)], axis=2)
        mixed = jnp.einsum('bngc,gce->bnge', pooled, w_grp)
        return mixed.reshape(t.shape) * scale
    return run(h), (run(hc) if need_ctx else None)


def gqa_mixer(h, hc, w_qkv, qk_norm, w_out, cos, sin, need_ctx):
    b = h.shape[0]
    scale = HEAD_DIM ** -0.5

    def project(t):
        n = t.shape[1]
        q, k, v = jnp.split(t @ w_qkv, [GQA_HEADS * HEAD_DIM, (GQA_HEADS + GQA_KV_HEADS) * HEAD_DIM], axis=-1)
        q = q.reshape(b, n, GQA_KV_HEADS, GQA_GROUP, HEAD_DIM).transpose(0, 2, 3, 1, 4)
        k = k.reshape(b, n, GQA_KV_HEADS, HEAD_DIM).transpose(0, 2, 1, 3)
        v = v.reshape(b, n, GQA_KV_HEADS, HEAD_DIM).transpose(0, 2, 1, 3)
        return rms_norm(q, qk_norm[0]), rms_norm(k, qk_norm[1]), v

    def attend(qb, k, v):
        s = jnp.einsum('bkgqd,bkld->bkgql', qb, k).astype(jnp.float32) * scale
        p = jax.nn.softmax(s, axis=-1).astype(v.dtype)
        return jnp.einsum('bkgql,bkld->bkgqd', p, v)

    def finish(o):
        return o.transpose(0, 3, 1, 2, 4).reshape(b, o.shape[3], -1) @ w_out

    q, k, v = project(h)
    qc, kc, vc = project(hc)
    q = apply_axial_rope(q, cos, sin)
    k = apply_axial_rope(k, cos, sin)
    k_all = jnp.concatenate([kc, k], axis=2)
    v_all = jnp.concatenate([vc, v], axis=2)
    out = finish(sweep_query_blocks(lambda qb: attend(qb, k_all, v_all), q))
    out_c = finish(attend(qc, kc, vc)) if need_ctx else None
    return out, out_c


def diff_mixer(h, hc, w_qkv, lam_vecs, subln, w_out, lam_init, cos, sin, need_ctx):
    b = h.shape[0]
    scale = HEAD_DIM ** -0.5
    lv = lam_vecs.astype(jnp.float32)
    lam = jnp.exp(jnp.sum(lv[0] * lv[1])) - jnp.exp(jnp.sum(lv[2] * lv[3])) + lam_init

    def project(t):
        n = t.shape[1]
        q, k, v = jnp.split(t @ w_qkv, 3, axis=-1)
        q = q.reshape(b, n, DIFF_HEADS, 2, HEAD_DIM).transpose(0, 2, 3, 1, 4)
        k = k.reshape(b, n, DIFF_HEADS, 2, HEAD_DIM).transpose(0, 2, 3, 1, 4)
        v = v.reshape(b, n, DIFF_HEADS, 2 * HEAD_DIM).transpose(0, 2, 1, 3)
        return q, k, v

    def attend(qb, k, v):
        s = jnp.einsum('bhmqd,bhmld->bhmql', qb, k).astype(jnp.float32) * scale
        p = jax.nn.softmax(s, axis=-1)
        a = p[:, :, 0] - lam * p[:, :, 1]
        return jnp.einsum('bhql,bhle->bhqe', a.astype(v.dtype), v)

    def finish(o):
        o = rms_norm(o, subln) * (1.0 - lam_init)
        return o.transpose(0, 2, 1, 3).reshape(b, o.shape[2], -1) @ w_out

    q, k, v = project(h)
    kc_q = project(hc)
    qc, kc, vc = kc_q
    q = apply_axial_rope(q, cos, sin)
    k = apply_axial_rope(k, cos, sin)
    k_all = jnp.concatenate([kc, k], axis=3)
    v_all = jnp.concatenate([vc, v], axis=2)
    out = finish(sweep_query_blocks(lambda qb: attend(qb, k_all, v_all), q))
    out_c = finish(attend(qc, kc, vc)) if need_ctx else None
    return out, out_c


def hier_moe(h, w_grp, b_grp, w_exp, b_exp, w_gate, w_up, w_down):
    b, n, _ = h.shape
    lg = (h @ w_grp).astype(jnp.float32) + b_grp
    g_star = jnp.argmax(lg, axis=-1)
    p_group = jnp.take_along_axis(jax.nn.softmax(lg, axis=-1), g_star[..., None], axis=-1)
    le = ((h @ w_exp).astype(jnp.float32) + b_exp).reshape(b, n, N_GROUPS, EXPERTS_PER_GROUP)
    le = jnp.take_along_axis(le, g_star[..., None, None], axis=2)[:, :, 0]
    top_v, top_i = lax.top_k(jax.nn.softmax(le, axis=-1), TOP_K)
    top_v = top_v / jnp.sum(top_v, axis=-1, keepdims=True)
    expert_idx = g_star[..., None] * EXPERTS_PER_GROUP + top_i
    combine = jnp.sum(jax.nn.one_hot(expert_idx, N_EXPERTS, dtype=jnp.float32)
                      * (p_group * top_v)[..., None], axis=-2)
    act = jax.nn.silu(jnp.einsum('bnd,edf->bnef', h, w_gate)) * jnp.einsum('bnd,edf->bnef', h, w_up)
    return jnp.einsum('bnef,efd->bnd', act * combine.astype(h.dtype)[..., None], w_down)


def setup_inputs(seed: int = 0) -> dict:
    key = jax.random.key(seed)
    ks = iter(jax.random.split(key, 32))
    d, L = D_MODEL, DEPTH
    nA, nB, nC, nD = _n_uses(0), _n_uses(1), _n_uses(2), _n_uses(3)

    def nrm(shape, std):
        return jax.random.normal(next(ks), shape, jnp.float32) * std

    return {
        'x': nrm((BATCH, SEQ, d), 1.0),
        'c': nrm((BATCH, d), 1.0),
        'ctx': nrm((BATCH, CTX_LEN, d), 1.0),
        'c_ctx': nrm((d,), 1.0),
        'mod_w': nrm((L, d, N_MOD * d), 0.5 * d ** -0.5),
        'mod_b': nrm((L, N_MOD * d), 0.02),
        'ln_g': 1.0 + nrm((L, 2, d), 0.05),
        'ln_b': nrm((L, 2, d), 0.02),
        'rt_grp_w': nrm((L, d, N_GROUPS), d ** -0.5),
        'rt_grp_b': nrm((L, N_GROUPS), 0.01),
        'rt_exp_w': nrm((L, d, N_EXPERTS), d ** -0.5),
        'rt_exp_b': nrm((L, N_EXPERTS), 0.01),
        'ex_w_gate': nrm((L, N_EXPERTS, d, EXPERT_FF), d ** -0.5),
        'ex_w_up': nrm((L, N_EXPERTS, d, EXPERT_FF), d ** -0.5),
        'ex_w_down': nrm((L, N_EXPERTS, EXPERT_FF, d), BETA * EXPERT_FF ** -0.5),
        'conv_in_w': nrm((nA, d, 3 * d), d ** -0.5),
        'conv_w': nrm((nA, 3, d), 3 ** -0.5),
        'conv_out_w': nrm((nA, d, d), BETA * d ** -0.5),
        'pool_w': nrm((nB, len(POOL_WINDOWS), POOL_GROUP, POOL_GROUP), BETA * POOL_GROUP ** -0.5),
        'pool_scale': 1.0 + nrm((nB, d), 0.1),
        'gqa_qkv_w': nrm((nC, d, GQA_QKV), d ** -0.5),
        'gqa_qk_norm': 1.0 + nrm((nC, 2, HEAD_DIM), 0.05),
        'gqa_out_w': nrm((nC, GQA_HEADS * HEAD_DIM, d), BETA * d ** -0.5),
        'diff_qkv_w': nrm((nD, d, 3 * d), d ** -0.5),
        'diff_lambda': nrm((nD, 4, HEAD_DIM), 0.1),
        'diff_subln': 1.0 + nrm((nD, 2 * HEAD_DIM), 0.05),
        'diff_out_w': nrm((nD, d, d), BETA * d ** -0.5),
    }


def reference(x, c, ctx, c_ctx, mod_w, mod_b, ln_g, ln_b, rt_grp_w, rt_grp_b, rt_exp_w, rt_exp_b,
              ex_w_gate, ex_w_up, ex_w_down, conv_in_w, conv_w, conv_out_w, pool_w, pool_scale,
              gqa_qkv_w, gqa_qk_norm, gqa_out_w, diff_qkv_w, diff_lambda, diff_subln, diff_out_w):
    rows = x.shape[1] // GRID_W
    cos, sin = axial_rope_tables(rows, x.dtype)
    s_lat = jax.nn.silu(c)[:, None, :]
    s_ctx = jax.nn.silu(c_ctx)[None, None, :]
    xc = ctx
    for i in range(DEPTH):
        mixer, j = i % N_MIXERS, i // N_MIXERS
        need_ctx = i < DEPTH - 1
        mod = jnp.split(s_lat @ mod_w[i] + mod_b[i], N_MOD, axis=-1)
        modc = jnp.split(s_ctx @ mod_w[i] + mod_b[i], N_MOD, axis=-1)
        h = modulate(x, mod[0], mod[1])
        hc = modulate(xc, modc[0], modc[1])
        if mixer == 0:
            o, oc = conv_mixer(h, hc, conv_in_w[j], conv_w[j], conv_out_w[j], need_ctx)
        elif mixer == 1:
            o, oc = pool_mixer(h, hc, pool_w[j], pool_scale[j], need_ctx)
        elif mixer == 2:
            o, oc = gqa_mixer(h, hc, gqa_qkv_w[j], gqa_qk_norm[j], gqa_out_w[j], cos, sin, need_ctx)
        else:
            lam_init = 0.8 - 0.6 * math.exp(-0.3 * i)
            o, oc = diff_mixer(h, hc, diff_qkv_w[j], diff_lambda[j], diff_subln[j], diff_out_w[j],
                               lam_init, cos, sin, need_ctx)
        moe_w = (rt_grp_w[i], rt_grp_b[i], rt_exp_w[i], rt_exp_b[i], ex_w_gate[i], ex_w_up[i], ex_w_down[i])
        x = layer_norm(ALPHA * x + mod[2] * o, ln_g[i, 0], ln_b[i, 0])
        x = layer_norm(ALPHA * x + mod[5] * hier_moe(modulate(x, mod[3], mod[4]), *moe_w),
                       ln_g[i, 1], ln_b[i, 1])
        if need_ctx:
            xc = layer_norm(ALPHA * xc + modc[2] * oc, ln_g[i, 0], ln_b[i, 0])
            xc = layer_norm(ALPHA * xc + modc[5] * hier_moe(modulate(xc, modc[3], modc[4]), *moe_w),
                            ln_g[i, 1], ln_b[i, 1])
    return x
```

```python
import math
import numpy as np
import concourse.bass as bass
import concourse.mybir as mybir
from concourse.bass_utils import run_bass_kernel_spmd

F32 = mybir.dt.float32
F32R = mybir.dt.float32r
BF16 = mybir.dt.bfloat16
AF = mybir.ActivationFunctionType
ALU = mybir.AluOpType
AX = mybir.AxisListType

D = 2048
NCH = 16
SEQ = 8192
CTX = 256
NCORE = 8
LAT_PC = SEQ // NCORE
CTX_PC = CTX // NCORE
HALO = 9
NLA = LAT_PC + 2 * HALO
NCA = CTX_PC + 2 * HALO
TA = NLA + NCA
TB = LAT_PC + CTX_PC
DEPTH = 4
ALPHA = (2.0 * DEPTH) ** 0.25
LN_EPS = 1e-6
RMS_EPS = 1e-6
NE = 16
FF = 768
NF = FF // 128
GRID_W = 64
HD = 128
SLOT = 4096
RING = 5
NSCR = 3
SAME_ENG_SYNC = True


class Buf:
    __slots__ = ("w", "r", "dsem", "dcnt", "name")

    def __init__(self, name=""):
        self.w = None
        self.r = []
        self.dsem = None
        self.dcnt = 0
        self.name = name


class Eng:
    def __init__(self, P, name, e, has_sem=True):
        self.name = name
        self.e = e
        self.sem = P.nc.alloc_semaphore("sem_" + name) if has_sem else None
        self.cnt = 0
        self.seen = {}
        self.ispe = name == "pe"
        self.last_signaled = True


class Prog:
    def __init__(self, nc):
        self.nc = nc
        self.pe = Eng(self, "pe", nc.tensor)
        self.act = Eng(self, "act", nc.scalar)
        self.dve = Eng(self, "dve", nc.vector)
        self.pool = Eng(self, "pool", nc.gpsimd)
        self.sp = Eng(self, "sp", nc.sync, has_sem=False)
        self.sems = []
        self.nwait = 0
        self.ninst = 0

    def _deps(self, reads, writes):
        deps = {}

        def add(ev):
            if ev is None:
                return
            k = id(ev[0])
            if k not in deps or deps[k][1] < ev[1]:
                deps[k] = ev

        for b in reads:
            add(b.w)
        for b in writes:
            add(b.w)
            for ev in b.r:
                add(ev)
        return deps

    def _wait(self, E, deps):
        for k, (sem, val) in deps.items():
            if sem is E.sem and (E.ispe or not SAME_ENG_SYNC):
                continue
            if E.seen.get(k, 0) >= val:
                continue
            E.e.wait_ge(sem, val)
            E.seen[k] = val
            self.nwait += 1

    def op(self, E, fn, reads=(), writes=(), signal=True):
        self._wait(E, self._deps(reads, writes))
        ins = fn(E.e)
        self.ninst += 1
        if signal:
            E.cnt += 1
            ins.then_inc(E.sem, 1)
            ev = (E.sem, E.cnt)
            E.last_signaled = True
        else:
            ev = (E.sem, E.cnt + 1)
            E.last_signaled = False
        for b in reads:
            b.r.append(ev)
        for b in writes:
            b.w = ev
            b.r = []
        return ins

    def dma(self, Q, out, in_, reads=(), writes=(), dbuf=None, **kw):
        self._wait(Q, self._deps(reads, writes))
        ins = Q.e.dma_start(out=out, in_=in_, **kw)
        self.ninst += 1
        b = dbuf
        if b.dsem is None:
            b.dsem = self.nc.alloc_semaphore("dsem%d" % len(self.sems))
            self.sems.append(b.dsem)
        b.dcnt += 16
        ins.then_inc(b.dsem, 16)
        ev = (b.dsem, b.dcnt)
        for r in reads:
            r.r.append(ev)
        for w in writes:
            w.w = ev
            w.r = []
        return ev

    def fix(self, bufs, dbuf):
        ev = (dbuf.dsem, dbuf.dcnt)
        for b in bufs:
            b.w = ev

    def barrier(self, bufs=()):
        engs = [self.pe, self.act, self.dve, self.pool]
        assert self.pe.last_signaled
        deps = self._deps((), bufs)
        for E in engs + [self.sp]:
            d = dict(deps)
            for F in engs:
                if F is not E and F.cnt > 0:
                    d[id(F.sem)] = (F.sem, F.cnt)
            self._wait(E, d)

    def wait_all(self, E, bufs):
        deps = self._deps((), bufs)
        self._wait(E, deps)


class TT:
    def __init__(self, P, name, shape, dtype, nbuf=1, psum=False, handle=None):
        if handle is not None:
            self.t = handle
        elif psum:
            self.t = P.nc.alloc_psum_tensor(name, shape, dtype)
        else:
            self.t = P.nc.alloc_sbuf_tensor(name, shape, dtype)
        self.b = [Buf(name + str(i)) for i in range(nbuf)]

    def __getitem__(self, idx):
        return self.t[idx]


class Phase:
    _n = 0

    def __init__(self, P):
        self.P = P
        self.guards = []
        self.tts = []

    def tt(self, name, shape, dtype, nbuf=1):
        Phase._n += 1
        g = self.P.nc.sbuf_tensor("%s_%d" % (name, Phase._n), shape, dtype)
        h = g.__enter__()
        self.guards.append(g)
        t = TT(self.P, name, shape, dtype, nbuf=nbuf, handle=h)
        self.tts.append(t)
        return t

    def close(self):
        P = self.P
        bufs = [b for t in self.tts for b in t.b]
        P.barrier(bufs)
        for g in reversed(self.guards):
            g.__exit__(None, None, None)


def chunked(v):
    v = np.asarray(v, np.float32).reshape(-1)
    n = v.shape[0] // 128
    return np.ascontiguousarray(v.reshape(n, 128).T)


def proj_slot(W, col_starts):
    out = np.zeros((128, 16, 256), np.float32)
    for i, c0 in enumerate(col_starts):
        out[:, :, i * 128:(i + 1) * 128] = W[:, c0:c0 + 128].reshape(16, 128, 128).transpose(1, 0, 2)
    return out.reshape(128, SLOT)


def build_slot(inp, spec):
    kind = spec[0]
    if kind == "mod":
        _, i, j = spec
        return proj_slot(inp["mod_w"][i], [j * 256, j * 256 + 128])
    if kind == "proj":
        _, name, j, cols = spec
        return proj_slot(inp[name][j], cols)
    if kind == "pool":
        _, g0 = spec
        out = np.zeros((128, 2, 4, 512), np.float32)
        for gi in range(2):
            out[:, gi] = inp["pool_w"][0][g0 + gi].reshape(4, 128, 512).transpose(1, 0, 2)
        return out.reshape(128, SLOT)
    if kind == "router":
        _, i = spec
        W = np.concatenate([inp["rt_grp_w"][i], inp["rt_exp_w"][i]], axis=1)
        out = np.zeros((128, SLOT), np.float32)
        out[:, :320] = W.reshape(16, 128, 20).transpose(1, 0, 2).reshape(128, 320)
        return out
    if kind == "up":
        _, i, e, f = spec
        out = np.zeros((128, 16, 256), np.float32)
        out[:, :, 0:128] = inp["ex_w_gate"][i, e][:, f * 128:(f + 1) * 128].reshape(16, 128, 128).transpose(1, 0, 2)
        out[:, :, 128:256] = inp["ex_w_up"][i, e][:, f * 128:(f + 1) * 128].reshape(16, 128, 128).transpose(1, 0, 2)
        return out.reshape(128, SLOT)
    if kind == "down":
        _, i, e, f0 = spec
        out = np.zeros((128, 2, 2048), np.float32)
        for k in range(2):
            out[:, k] = inp["ex_w_down"][i, e][(f0 + k) * 128:(f0 + k + 1) * 128, :]
        return out.reshape(128, SLOT)
    if kind == "vproj":
        _, name, j, c0, col0 = spec
        W = inp[name][j]
        out = W[c0 * 128:(c0 + 8) * 128, col0:col0 + 512].reshape(8, 128, 512).transpose(1, 0, 2)
        return np.ascontiguousarray(out).reshape(128, SLOT)
    raise ValueError(kind)


class PVec:
    def __init__(self):
        self.cols = []
        self.off = {}
        self.n = 0

    def add(self, name, arr):
        arr = np.asarray(arr, np.float32)
        assert arr.shape[0] == 128
        arr = arr.reshape(128, -1)
        self.off[name] = (self.n, arr.shape[1])
        self.cols.append(arr)
        self.n += arr.shape[1]

    def array(self):
        return np.ascontiguousarray(np.concatenate(self.cols, axis=1))


def make_pvec(inp, layers):
    pv = PVec()
    pv.add("c", chunked(inp["c"][0]))
    pv.add("cctx", chunked(inp["c_ctx"]))
    for i in layers:
        pv.add("mod_b%d" % i, chunked(inp["mod_b"][i]))
        for k in range(2):
            pv.add("ln_g%d_%d" % (i, k), chunked(inp["ln_g"][i, k]))
            pv.add("ln_b%d_%d" % (i, k), chunked(inp["ln_b"][i, k]))
        rb = np.concatenate([inp["rt_grp_b"][i], inp["rt_exp_b"][i]])
        pv.add("rtb%d" % i, np.broadcast_to(rb[None, :], (128, 20)))
    if 0 in layers:
        for k in range(3):
            pv.add("conv_w%d" % k, chunked(inp["conv_w"][0, k]))
    if 1 in layers:
        pv.add("pool_scale", chunked(inp["pool_scale"][0]))
    if 2 in layers:
        pv.add("qk_norm", np.ascontiguousarray(inp["gqa_qk_norm"][0].T))
    if 3 in layers:
        pv.add("lam", np.ascontiguousarray(inp["diff_lambda"][0].T))
        pv.add("subln", chunked(inp["diff_subln"][0]))
    return pv


def make_consts():
    c = np.zeros((128, 128 * 3), np.float32)
    c[:, 0:128] = np.eye(128, dtype=np.float32)
    c[:, 128:256] = 1.0
    L = np.zeros((128, 128), np.float32)
    for i in range(32):
        L[32 + i, i] = -1.0
        L[i, 32 + i] = 1.0
        L[96 + i, 64 + i] = -1.0
        L[64 + i, 96 + i] = 1.0
    c[:, 256:384] = L
    return c


C_IDENT, C_ONES, C_ROT, C_SEL = 0, 128, 256, 384


def plan_mod(i):
    return [("mod", i, j) for j in range(48)]


def plan_moe(i, side=None):
    p = [("router", i)]
    for e in range(NE):
        for f in range(NF):
            p.append(("up", i, e, f))
        for f0 in range(0, NF, 2):
            p.append(("down", i, e, f0))
        if side is not None:
            p += [("mod", side, e * 3 + j) for j in range(3)]
    return p


def plan_layer0():
    p = []
    for c in range(16):
        if c % 2 == 0:
            p.append(("proj", "conv_in_w", 0, [c * 128, (c + 1) * 128]))
        p.append(("proj", "conv_in_w", 0, [2048 + c * 128, 4096 + c * 128]))
    for d in range(0, 16, 2):
        p.append(("proj", "conv_out_w", 0, [d * 128, (d + 1) * 128]))
    return p


def plan_layer1():
    return [("pool", 0), ("pool", 2)]


class WStream:
    def __init__(self, P, plan, wdram):
        self.P = P
        self.plan = plan
        self.w = wdram
        self.issued = 0
        self.cur = 0
        self.released = [True] * RING
        self.didx = []
        n = 0
        for sp in plan:
            self.didx.append(n)
            if sp[0] != "fence":
                n += 1
        self._alloc()

    def _alloc(self):
        Phase._n += 1
        self.guard = self.P.nc.sbuf_tensor("wring_%d" % Phase._n, [128, RING, SLOT], BF16)
        h = self.guard.__enter__()
        self.ring = TT(self.P, "wring", [128, RING, SLOT], BF16, nbuf=RING, handle=h)
        self.released = [True] * RING

    def _pump(self):
        while self.issued < len(self.plan) and self.issued < self.cur + RING:
            j = self.issued
            if self.plan[j][0] == "fence":
                break
            s = j % RING
            if not self.released[s]:
                break
            b = self.ring.b[s]
            self.P.dma(self.P.pool, self.ring[:, s, :].rearrange("p (a b) -> p a b", b=2048),
                       self.w[self.didx[j]].rearrange("p (a b) -> p a b", b=2048), writes=[b], dbuf=b)
            self.released[s] = False
            self.issued += 1

    def get(self, kind):
        i = self.cur
        assert i < len(self.plan), "stream exhausted"
        assert self.plan[i][0] == kind, (self.plan[i], kind)
        self._pump()
        assert self.issued > i, "ring deadlock: slot not released"
        self.cur += 1
        s = i % RING
        return s, self.ring.b[s]

    def release(self, slot):
        self.released[slot[0]] = True
        self._pump()

    def suspend(self):
        assert self.plan[self.cur][0] == "fence" and self.issued == self.cur, (self.cur, self.issued)
        assert all(self.released)
        self.P.barrier(self.ring.b)
        self.guard.__exit__(None, None, None)

    def resume(self):
        self._alloc()
        self.cur += 1
        self.issued += 1


def stream_array(inp, plan):
    specs = [sp for sp in plan if sp[0] != "fence"]
    ws = np.empty((len(specs), 128, SLOT), np.float32)
    for n, spec in enumerate(specs):
        ws[n] = build_slot(inp, spec)
    return ws


class K:
    pass


def tok_tiles(T, n=3):
    w = (T + n - 1) // n
    return [(i * w, min(T, (i + 1) * w)) for i in range(n) if i * w < T]


def cls_split(lo, hi, nl):
    out = []
    if lo < nl:
        out.append((lo, min(hi, nl), 0))
    if hi > nl:
        out.append((max(lo, nl), hi, 1))
    return out


def setup_common(P, k, T, nl, pv, layers, pvd, cstd):
    nc = P.nc
    k.T, k.nl = T, nl
    k.tiles = tok_tiles(T)
    k.pv = pv
    k.PV = TT(P, "PV", [128, pv.n], F32)
    k.CST = TT(P, "CST", [128, 384], F32)
    ld = Buf("ld")
    P.dma(P.sp, k.PV[:, :], pvd, writes=[k.PV.b[0]], dbuf=ld)
    P.dma(P.sp, k.CST[:, :], cstd, writes=[k.CST.b[0]], dbuf=ld)
    P.fix([k.PV.b[0], k.CST.b[0]], ld)
    k.X = TT(P, "X", [128, NCH, T], F32, nbuf=NCH)
    k.H = TT(P, "H", [128, NCH, T], BF16, nbuf=NCH)
    k.PS = [TT(P, "ps%d" % i, [128, 512], F32, psum=True) for i in range(8)]
    k.psi = 0
    k.ps_res = set()
    k.MOD = TT(P, "MOD", [128, len(layers), 2, 96], F32)
    k.DER = TT(P, "DER", [128, len(layers), 2, 6, 16], F32)
    k.lidx = {l: n for n, l in enumerate(layers)}
    k.SCR = TT(P, "SCR", [128, NSCR, 512], F32, nbuf=NSCR)
    k.scri = 0
    k.zni = 0
    k.EPS = TT(P, "EPS", [128, 4], F32)
    k.IDB = TT(P, "IDB", [128, 128], BF16)
    k.ONEB = TT(P, "ONEB", [128, 128], BF16)
    k.SBT = TT(P, "SBT", [128, NCH, 2], BF16)
    P.op(P.dve, lambda e: e.memset(k.EPS[:, 0:1], LN_EPS / (ALPHA * ALPHA)), writes=[k.EPS.b[0]])
    P.op(P.dve, lambda e: e.memset(k.EPS[:, 1:2], RMS_EPS), writes=[k.EPS.b[0]])
    P.op(P.dve, lambda e: e.memset(k.EPS[:, 2:3], 0.0), writes=[k.EPS.b[0]])
    P.op(P.dve, lambda e: e.tensor_copy(out=k.IDB[:, :], in_=k.CST[:, C_IDENT:C_IDENT + 128]),
         reads=[k.CST.b[0]], writes=[k.IDB.b[0]])
    P.op(P.dve, lambda e: e.tensor_copy(out=k.ONEB[:, :], in_=k.CST[:, C_ONES:C_ONES + 128]),
         reads=[k.CST.b[0]], writes=[k.ONEB.b[0]])
    k.ONER = TT(P, "ONER", [128, 128], F32)
    P.op(P.dve, lambda e: e.tensor_copy(out=k.ONER[:, :].bitcast(F32R), in_=k.CST[:, C_ONES:C_ONES + 128]),
         reads=[k.CST.b[0]], writes=[k.ONER.b[0]])


def psum(k):
    while True:
        t = k.PS[k.psi % 8]
        k.psi += 1
        if id(t) not in k.ps_res:
            return t


def ps_reserve(k):
    t = psum(k)
    k.ps_res.add(id(t))
    return t


def ps_unreserve(k, t):
    k.ps_res.discard(id(t))


def scr(k):
    i = k.scri % NSCR
    k.scri += 1
    return k.SCR[:, i, :], k.SCR.b[i]


def pvs(k, name, lo=None, hi=None):
    o, w = k.pv.off[name]
    if lo is None:
        return k.PV[:, o:o + w]
    return k.PV[:, o + lo:o + hi]


def emit_mod(P, k, W, i):
    for _ in mod_steps(P, k, W, i):
        pass


def mod_steps(P, k, W, i):
    li = k.lidx[i]
    ps = ps_reserve(k)
    for j2 in range(48):
        s, sb = W.get("mod")
        for jj in range(2):
            j = j2 * 2 + jj
            for c in range(NCH):
                P.op(P.pe, lambda e, c=c, jj=jj, j=j, s=s: e.matmul(
                    ps[:, 2 * j:2 * j + 2], W.ring[:, s, c * 256 + jj * 128:c * 256 + jj * 128 + 128],
                    k.SBT[:, c, :],
                    start=(c == 0), stop=(c == NCH - 1)),
                    reads=[sb, k.SBT.b[0]], writes=[ps.b[0]], signal=(c == NCH - 1))
        W.release((s, sb))
        yield
    o, w = k.pv.off["mod_b%d" % i]
    for cls in range(2):
        P.op(P.dve, lambda e, cls=cls: e.tensor_tensor(
            out=k.MOD[:, li, cls, :], in0=ps[:, 0:192].rearrange("p (j c) -> p j c", c=2)[:, :, cls],
            in1=k.PV[:, o:o + 96], op=ALU.add),
            reads=[ps.b[0], k.PV.b[0]], writes=[k.MOD.b[0]])
    g1 = pvs(k, "ln_g%d_0" % i)
    b1 = pvs(k, "ln_b%d_0" % i)
    for cls in range(2):
        M = lambda m: k.MOD[:, li, cls, m * 16:(m + 1) * 16]
        Dv = lambda n: k.DER[:, li, cls, n, :]
        rw = dict(reads=[k.MOD.b[0], k.PV.b[0], k.DER.b[0]], writes=[k.DER.b[0]])
        P.op(P.dve, lambda e: e.tensor_scalar(out=Dv(0), in0=M(1), scalar1=1.0, scalar2=None, op0=ALU.add), **rw)
        P.op(P.dve, lambda e: e.tensor_copy(out=Dv(1), in_=M(0)), **rw)
        P.op(P.dve, lambda e: e.tensor_scalar(out=Dv(2), in0=M(2), scalar1=1.0 / ALPHA, scalar2=None, op0=ALU.mult), **rw)
        P.op(P.dve, lambda e: e.scalar_tensor_tensor(out=Dv(3), in0=M(4), scalar=1.0, in1=g1, op0=ALU.add, op1=ALU.mult), **rw)
        P.op(P.dve, lambda e: e.scalar_tensor_tensor(out=Dv(4), in0=M(4), scalar=1.0, in1=b1, op0=ALU.add, op1=ALU.mult), **rw)
        P.op(P.dve, lambda e: e.tensor_tensor(out=Dv(4), in0=Dv(4), in1=M(3), op=ALU.add), **rw)
        P.op(P.dve, lambda e: e.tensor_scalar(out=Dv(5), in0=M(5), scalar1=1.0 / ALPHA, scalar2=None, op0=ALU.mult), **rw)
    ps_unreserve(k, ps)
    yield


def emit_silu_c(P, k):
    for cls, name in enumerate(["c", "cctx"]):
        P.op(P.act, lambda e, cls=cls, name=name: e.activation(out=k.SBT[:, :, cls], in_=pvs(k, name), func=AF.Silu),
             reads=[k.PV.b[0]], writes=[k.SBT.b[0]])


def emit_modulate(P, k, i):
    li = k.lidx[i]
    for c in range(NCH):
        for (lo, hi, cls) in cls_split(0, k.T, k.nl):
            eng = P.dve if (c % 2 == 0) else P.pool
            P.op(eng, lambda e, c=c, lo=lo, hi=hi, cls=cls: e.tensor_scalar(
                out=k.H[:, c, lo:hi], in0=k.X[:, c, lo:hi],
                scalar1=k.DER[:, li, cls, 0, c:c + 1], scalar2=k.DER[:, li, cls, 1, c:c + 1],
                op0=ALU.mult, op1=ALU.add),
                reads=[k.X.b[c], k.DER.b[0]], writes=[k.H.b[c]])


def emit_ln(P, k, i, which, make_h):
    li = k.lidx[i]
    onesr = k.ONER[:, :].bitcast(F32R)
    T = k.T
    ph = Phase(P)
    k.MEAN = ph.tt("MEAN", [128, T], F32)
    k.RSTD = ph.tt("RSTD", [128, T], F32)
    k.ZN = ph.tt("ZN", [128, 2, T], F32, nbuf=2)
    SQ = ph.tt("LSQ", [128, 2, 512], F32, nbuf=2)
    XR = ph.tt("LXR", [128, 2, 512], F32, nbuf=2)
    for (lo, hi) in k.tiles:
        n = hi - lo
        ps1, ps2 = psum(k), psum(k)
        for c in range(NCH):
            z = c % 2
            P.op(P.act, lambda e, c=c, z=z: e.activation(out=SQ[:, z, :n].bitcast(F32R), in_=k.X[:, c, lo:hi], func=AF.Square),
                 reads=[k.X.b[c]], writes=[SQ.b[z]])
            eng = P.dve if c % 2 == 0 else P.pool
            P.op(eng, lambda e, c=c, z=z: e.tensor_copy(out=XR[:, z, :n].bitcast(F32R), in_=k.X[:, c, lo:hi]),
                 reads=[k.X.b[c]], writes=[XR.b[z]])
            P.op(P.pe, lambda e, c=c, z=z: e.matmul(ps1[:, :n], onesr, XR[:, z, :n].bitcast(F32R), start=(c == 0), stop=(c == NCH - 1)),
                 reads=[XR.b[z], k.ONER.b[0]], writes=[ps1.b[0]], signal=True)
            P.op(P.pe, lambda e, c=c, z=z: e.matmul(ps2[:, :n], onesr, SQ[:, z, :n].bitcast(F32R), start=(c == 0), stop=(c == NCH - 1)),
                 reads=[SQ.b[z], k.ONER.b[0]], writes=[ps2.b[0]], signal=True)
        P.op(P.act, lambda e: e.mul(out=k.MEAN[:, lo:hi], in_=ps1[:, :n], mul=1.0 / D),
             reads=[ps1.b[0]], writes=[k.MEAN.b[0]])
        msq, msqb = scr(k)
        P.op(P.dve, lambda e: e.tensor_tensor(out=msq[:, :n], in0=k.MEAN[:, lo:hi], in1=k.MEAN[:, lo:hi], op=ALU.mult),
             reads=[k.MEAN.b[0]], writes=[msqb])
        P.op(P.dve, lambda e: e.scalar_tensor_tensor(out=msq[:, :n], in0=ps2[:, :n], scalar=1.0 / D, in1=msq[:, :n],
                                                     op0=ALU.mult, op1=ALU.subtract),
             reads=[ps2.b[0], msqb], writes=[msqb])
        P.op(P.act, lambda e: e.activation(out=msq[:, :n], in_=msq[:, :n], func=AF.Sqrt, bias=k.EPS[:, 0:1]),
             reads=[msqb, k.EPS.b[0]], writes=[msqb])
        P.op(P.dve, lambda e: e.reciprocal(out=k.RSTD[:, lo:hi], in_=msq[:, :n]),
             reads=[msqb], writes=[k.RSTD.b[0]])
    g = pvs(k, "ln_g%d_%d" % (i, which))
    b = pvs(k, "ln_b%d_%d" % (i, which))
    for c in range(NCH):
        z = k.zni % 2
        k.zni += 1
        e1 = P.dve if c % 4 == 3 else P.pool
        P.op(e1, lambda e, c=c, z=z: e.tensor_tensor(out=k.ZN[:, z, :], in0=k.X[:, c, 0:T], in1=k.MEAN[:, :], op=ALU.subtract),
             reads=[k.X.b[c], k.MEAN.b[0]], writes=[k.ZN.b[z]])
        P.op(P.dve, lambda e, z=z: e.tensor_tensor(out=k.ZN[:, z, :], in0=k.ZN[:, z, :], in1=k.RSTD[:, :], op=ALU.mult),
             reads=[k.ZN.b[z], k.RSTD.b[0]], writes=[k.ZN.b[z]])
        P.op(P.act, lambda e, c=c, z=z: e.activation(out=k.X[:, c, 0:T], in_=k.ZN[:, z, :], func=AF.Identity,
                                                     scale=g[:, c:c + 1], bias=b[:, c:c + 1]),
             reads=[k.ZN.b[z], k.PV.b[0]], writes=[k.X.b[c]])
        if make_h:
            for (lo, hi, cls) in cls_split(0, T, k.nl):
                P.op(P.act, lambda e, c=c, z=z, lo=lo, hi=hi, cls=cls: e.activation(
                    out=k.H[:, c, lo:hi], in_=k.ZN[:, z, lo:hi], func=AF.Identity,
                    scale=k.DER[:, li, cls, 3, c:c + 1], bias=k.DER[:, li, cls, 4, c:c + 1]),
                    reads=[k.ZN.b[z], k.DER.b[0]], writes=[k.H.b[c]])
    ph.close()


def emit_resid(P, k, ps, d, lo, hi, scal_fn, extra_reads=()):
    for (a, b_, cls) in cls_split(lo, hi, k.nl):
        P.op(P.dve, lambda e, a=a, b_=b_, cls=cls: e.scalar_tensor_tensor(
            out=k.X[:, d, a:b_], in0=ps[:, a - lo:b_ - lo], scalar=scal_fn(cls, d), in1=k.X[:, d, a:b_],
            op0=ALU.mult, op1=ALU.add),
            reads=[ps.b[0], k.X.b[d], k.DER.b[0]] + list(extra_reads), writes=[k.X.b[d]])


def emit_moe(P, k, W, i, side=None):
    li = k.lidx[i]
    T = k.T
    nt128 = (T + 127) // 128
    ph = Phase(P)
    k.COMBT = ph.tt("COMBT", [16, T], F32)
    k.CM = ph.tt("CM", [16, T], F32)
    k.CB = ph.tt("CB", [128, T], F32)
    k.ACTB = ph.tt("ACTB", [128, NF, T], BF16, nbuf=NF)
    s, sb = W.get("router")
    rtb = pvs(k, "rtb%d" % i)
    ident = k.CST[:, C_IDENT:C_IDENT + 128]
    NRG = 5
    RT = ph.tt("RT", [128, NRG, 96], F32, nbuf=NRG)

    def route_tile(t):
        lo, hi = t * 128, min(T, (t + 1) * 128)
        n = hi - lo
        ps = psum(k)
        for c in range(NCH):
            P.op(P.pe, lambda e, c=c: e.matmul(ps[:n, 0:20], k.H[:, c, lo:hi], W.ring[:, s, c * 20:(c + 1) * 20],
                                               start=(c == 0), stop=(c == NCH - 1)),
                 reads=[k.H.b[c], sb], writes=[ps.b[0]], signal=(c == NCH - 1))
        R, Rb = RT[:, t % NRG, :], RT.b[t % NRG]
        yield
        rw = dict(reads=[Rb], writes=[Rb])
        yield
        LG = R[:n, 0:20]
        P.op(P.dve, lambda e: e.tensor_tensor(out=LG, in0=ps[:n, 0:20], in1=rtb[:n, :], op=ALU.add),
             reads=[ps.b[0], k.PV.b[0]], writes=[Rb])
        yield
        gmax = R[:n, 20:21]
        P.op(P.dve, lambda e: e.tensor_reduce(out=gmax, in_=R[:n, 0:4], axis=AX.X, op=ALU.max), **rw)
        yield
        oh = R[:n, 24:28]
        P.op(P.dve, lambda e: e.tensor_scalar(out=oh, in0=R[:n, 0:4], scalar1=gmax, scalar2=None, op0=ALU.is_ge), **rw)
        yield
        ngm = R[:n, 21:22]
        P.op(P.dve, lambda e: e.tensor_scalar(out=ngm, in0=gmax, scalar1=-1.0, scalar2=None, op0=ALU.mult), **rw)
        yield
        ex = R[:n, 28:32]
        ssum = R[:n, 22:23]
        P.op(P.act, lambda e: e.activation(out=ex, in_=R[:n, 0:4], func=AF.Exp, bias=ngm, accum_out=ssum), **rw)
        yield
        pg = R[:n, 23:24]
        P.op(P.dve, lambda e: e.reciprocal(out=pg, in_=ssum), **rw)
        yield
        les = R[:n, 48:52]
        P.op(P.dve, lambda e: e.tensor_scalar(out=les, in0=R[:n, 4:8], scalar1=R[:n, 24:25], scalar2=None, op0=ALU.mult), **rw)
        yield
        for g in range(1, 4):
            P.op(P.dve, lambda e, g=g: e.scalar_tensor_tensor(out=les, in0=R[:n, 4 + 4 * g:8 + 4 * g], scalar=R[:n, 24 + g:25 + g],
                                                              in1=les, op0=ALU.mult, op1=ALU.add), **rw)
        m1 = R[:n, 52:53]
        P.op(P.dve, lambda e: e.tensor_reduce(out=m1, in_=les, axis=AX.X, op=ALU.max), **rw)
        yield
        k1 = R[:n, 56:60]
        P.op(P.dve, lambda e: e.tensor_scalar(out=k1, in0=les, scalar1=m1, scalar2=None, op0=ALU.is_ge), **rw)
        yield
        le2 = R[:n, 60:64]
        P.op(P.dve, lambda e: e.scalar_tensor_tensor(out=le2, in0=k1, scalar=-1e30, in1=les, op0=ALU.mult, op1=ALU.add), **rw)
        yield
        m2 = R[:n, 53:54]
        P.op(P.dve, lambda e: e.tensor_reduce(out=m2, in_=le2, axis=AX.X, op=ALU.max), **rw)
        yield
        k2 = R[:n, 64:68]
        P.op(P.dve, lambda e: e.tensor_scalar(out=k2, in0=le2, scalar1=m2, scalar2=None, op0=ALU.is_ge), **rw)
        yield
        dm = R[:n, 54:55]
        P.op(P.dve, lambda e: e.tensor_tensor(out=dm, in0=m2, in1=m1, op=ALU.subtract), **rw)
        yield
        w2 = R[:n, 55:56]
        P.op(P.act, lambda e: e.activation(out=w2, in_=dm, func=AF.Sigmoid), **rw)
        yield
        w1 = R[:n, 68:69]
        P.op(P.dve, lambda e: e.tensor_scalar(out=w1, in0=w2, scalar1=-1.0, scalar2=1.0, op0=ALU.mult, op1=ALU.add), **rw)
        yield
        cs = R[:n, 72:76]
        P.op(P.dve, lambda e: e.tensor_scalar(out=cs, in0=k1, scalar1=w1, scalar2=None, op0=ALU.mult), **rw)
        yield
        P.op(P.dve, lambda e: e.scalar_tensor_tensor(out=cs, in0=k2, scalar=w2, in1=cs, op0=ALU.mult, op1=ALU.add), **rw)
        yield
        P.op(P.dve, lambda e: e.tensor_scalar(out=cs, in0=cs, scalar1=pg, scalar2=None, op0=ALU.mult), **rw)
        yield
        comb = R[:n, 80:96]
        for g in range(4):
            P.op(P.dve, lambda e, g=g: e.tensor_scalar(out=R[:n, 80 + 4 * g:84 + 4 * g], in0=cs, scalar1=R[:n, 24 + g:25 + g],
                                                       scalar2=None, op0=ALU.mult), **rw)
        pst = psum(k)
        P.op(P.pe, lambda e: e.transpose(pst[:16, :n], comb, ident[:n, :n]),
             reads=[Rb, k.CST.b[0]], writes=[pst.b[0]])
        yield
        P.op(P.act, lambda e: e.copy(out=k.COMBT[:16, lo:hi], in_=pst[:16, :n]),
             reads=[pst.b[0]], writes=[k.COMBT.b[0]])
        yield

    for g0 in range(0, nt128, NRG):
        gens = [route_tile(t) for t in range(g0, min(nt128, g0 + NRG))]
        while gens:
            for g_ in list(gens):
                try:
                    next(g_)
                except StopIteration:
                    gens.remove(g_)
    W.release((s, sb))
    for ex_ in range(NE):
        P.op(P.dve, lambda e: e.tensor_scalar(out=k.CM[:16, :], in0=k.COMBT[:16, :],
                                              scalar1=k.CST[0:16, C_IDENT + ex_:C_IDENT + ex_ + 1], scalar2=None, op0=ALU.mult),
             reads=[k.COMBT.b[0], k.CST.b[0]], writes=[k.CM.b[0]])
        for (lo, hi) in k.tiles:
            n = hi - lo
            ps = psum(k)
            P.op(P.pe, lambda e: e.matmul(ps[:, :n], k.CST[0:16, C_ONES:C_ONES + 128], k.CM[:16, lo:hi],
                                          start=True, stop=True),
                 reads=[k.CST.b[0], k.CM.b[0]], writes=[ps.b[0]])
            P.op(P.act, lambda e: e.copy(out=k.CB[:, lo:hi], in_=ps[:, :n]), reads=[ps.b[0]], writes=[k.CB.b[0]])
        for f in range(NF):
            s, sb = W.get("up")
            for (lo, hi) in k.tiles:
                n = hi - lo
                psg, psu = psum(k), psum(k)
                for which, ps in ((0, psg), (1, psu)):
                    for c in range(NCH):
                        P.op(P.pe, lambda e, c=c, which=which, ps=ps: e.matmul(
                            ps[:, :n], W.ring[:, s, c * 256 + which * 128:c * 256 + which * 128 + 128], k.H[:, c, lo:hi],
                            start=(c == 0), stop=(c == NCH - 1)),
                            reads=[sb, k.H.b[c]], writes=[ps.b[0]], signal=(c == NCH - 1))
                sg, sgb = scr(k)
                P.op(P.act, lambda e: e.activation(out=sg[:, :n], in_=psg[:, :n], func=AF.Silu),
                     reads=[psg.b[0]], writes=[sgb])
                P.op(P.pool, lambda e: e.tensor_tensor(out=sg[:, :n], in0=sg[:, :n], in1=k.CB[:, lo:hi], op=ALU.mult),
                     reads=[sgb, k.CB.b[0]], writes=[sgb])
                P.op(P.dve, lambda e: e.tensor_tensor(out=k.ACTB[:, f, lo:hi], in0=psu[:, :n], in1=sg[:, :n], op=ALU.mult),
                     reads=[psu.b[0], sgb], writes=[k.ACTB.b[f]])
            W.release((s, sb))
        slots = [W.get("down") for _ in range(NF // 2)]
        for d in range(NCH):
            for (lo, hi) in k.tiles:
                n = hi - lo
                ps = psum(k)
                for f in range(NF):
                    s, sb = slots[f // 2]
                    off = (f % 2) * 2048 + d * 128
                    P.op(P.pe, lambda e, f=f, s=s, off=off: e.matmul(
                        ps[:, :n], W.ring[:, s, off:off + 128], k.ACTB[:, f, lo:hi], start=(f == 0), stop=(f == NF - 1)),
                        reads=[sb, k.ACTB.b[f]], writes=[ps.b[0]], signal=(f == NF - 1))
                emit_resid(P, k, ps, d, lo, hi, lambda cls, d: k.DER[:, li, cls, 5, d:d + 1])
        for sl in slots:
            W.release(sl)
        if side is not None:
            for _ in range(3):
                next(side, None)
    if side is not None:
        for _ in side:
            pass
    ph.close()


def emit_conv_mixer(P, k, W, i):
    li = k.lidx[i]
    T = k.T
    ph = Phase(P)
    G = ph.tt("G", [128, NCH, T], BF16, nbuf=NCH)
    k.UM = ph.tt("UM", [128, T], F32)
    k.CV = ph.tt("CV", [128, T], F32)
    P.op(P.pool, lambda e: e.memset(k.CV[:, :], 0.0), writes=[k.CV.b[0]])
    gb_slot = None
    for c in range(NCH):
        if c % 2 == 0:
            gb_slot = W.get("proj")
        s, sb = W.get("proj")
        UM = k.UM
        for (lo, hi) in k.tiles:
            n = hi - lo
            psc, psv = psum(k), psum(k)
            for which, ps in ((0, psc), (1, psv)):
                for cc in range(NCH):
                    P.op(P.pe, lambda e, cc=cc, which=which, ps=ps: e.matmul(
                        ps[:, :n], W.ring[:, s, cc * 256 + which * 128:cc * 256 + which * 128 + 128], k.H[:, cc, lo:hi],
                        start=(cc == 0), stop=(cc == NCH - 1)),
                        reads=[sb, k.H.b[cc]], writes=[ps.b[0]], signal=(cc == NCH - 1))
            vm, vmb = scr(k)
            P.op(P.dve, lambda e: e.tensor_tensor(out=vm[:, :n], in0=psv[:, :n], in1=k.VM[:, lo:hi], op=ALU.mult),
                 reads=[psv.b[0], k.VM.b[0]], writes=[vmb])
            P.op(P.dve, lambda e: e.tensor_tensor(out=UM[:, lo:hi], in0=psc[:, :n], in1=vm[:, :n], op=ALU.mult),
                 reads=[psc.b[0], vmb], writes=[UM.b[0]])
        W.release((s, sb))
        CV = k.CV
        w0 = pvs(k, "conv_w0")[:, c:c + 1]
        w1 = pvs(k, "conv_w1")[:, c:c + 1]
        w2 = pvs(k, "conv_w2")[:, c:c + 1]
        rw = dict(reads=[UM.b[0], k.PV.b[0], CV.b[0]], writes=[CV.b[0]])
        P.op(P.pool, lambda e: e.tensor_scalar(out=CV[:, 1:T - 1], in0=UM[:, 0:T - 2], scalar1=w0, scalar2=None, op0=ALU.mult), **rw)
        P.op(P.dve, lambda e: e.scalar_tensor_tensor(out=CV[:, 1:T - 1], in0=UM[:, 1:T - 1], scalar=w1, in1=CV[:, 1:T - 1],
                                                     op0=ALU.mult, op1=ALU.add), **rw)
        P.op(P.dve, lambda e: e.scalar_tensor_tensor(out=CV[:, 1:T - 1], in0=UM[:, 2:T], scalar=w2, in1=CV[:, 1:T - 1],
                                                     op0=ALU.mult, op1=ALU.add), **rw)
        s2, sb2 = gb_slot
        which = c % 2
        for (lo, hi) in k.tiles:
            n = hi - lo
            ps = psum(k)
            for cc in range(NCH):
                P.op(P.pe, lambda e, cc=cc: e.matmul(
                    ps[:, :n], W.ring[:, s2, cc * 256 + which * 128:cc * 256 + which * 128 + 128], k.H[:, cc, lo:hi],
                    start=(cc == 0), stop=(cc == NCH - 1)),
                    reads=[sb2, k.H.b[cc]], writes=[ps.b[0]], signal=(cc == NCH - 1))
            P.op(P.dve, lambda e: e.tensor_tensor(out=G[:, c, lo:hi], in0=ps[:, :n], in1=CV[:, lo:hi], op=ALU.mult),
                 reads=[ps.b[0], CV.b[0]], writes=[G.b[c]])
        if c % 2 == 1:
            W.release(gb_slot)
    for d2 in range(0, NCH, 2):
        s, sb = W.get("proj")
        for which in range(2):
            d = d2 + which
            for (lo, hi) in k.tiles:
                n = hi - lo
                ps = psum(k)
                for cc in range(NCH):
                    P.op(P.pe, lambda e, cc=cc: e.matmul(
                        ps[:, :n], W.ring[:, s, cc * 256 + which * 128:cc * 256 + which * 128 + 128], G[:, cc, lo:hi],
                        start=(cc == 0), stop=(cc == NCH - 1)),
                        reads=[sb, G.b[cc]], writes=[ps.b[0]], signal=(cc == NCH - 1))
                emit_resid(P, k, ps, d, lo, hi, lambda cls, d: k.DER[:, li, cls, 2, d:d + 1])
        W.release((s, sb))
    ph.close()


POOL_WINDOWS = (2, 4, 8, 16)


def emit_pool_mixer(P, k, W, i):
    li = k.lidx[i]
    T = k.T
    ph = Phase(P)
    RC = ph.tt("RC", [128, T], F32)
    A = ph.tt("PA", [128, T], F32)
    Bt = ph.tt("PB", [128, T], F32)
    HM = ph.tt("HM", [128, T], F32)
    WS = ph.tt("WS", [128, T], F32)
    PSC = ph.tt("PSC", [128, 2, 16], F32)
    VM = k.VM
    for t_ in (RC, A, Bt, WS):
        P.op(P.pool, lambda e, t_=t_: e.memset(t_[:, :], 0.0), writes=[t_.b[0]])

    def wsum(eng, dst, src, widx):
        cur = src
        for st in range(widx + 1):
            o = dst if st == widx else (A if st % 2 == 0 else Bt)
            lo, hi = 1 << st, T - (1 << st) + 1
            if st == 0:
                in0, in1 = cur[:, 0:T - 1], cur[:, 1:T]
            else:
                sh = 1 << (st - 1)
                in0, in1 = cur[:, lo - sh:hi - sh], cur[:, lo + sh:hi + sh]
            P.op(eng, lambda e: e.tensor_tensor(out=o[:, lo:hi], in0=in0, in1=in1, op=ALU.add),
                 reads=[cur.b[0]], writes=[o.b[0]])
            cur = o

    for c in range(NCH):
        widx = c // 4
        if c % 4 == 0:
            wsum(P.pool, RC, VM, widx)
            P.op(P.dve, lambda e: e.tensor_scalar(out=RC[:, :], in0=RC[:, :], scalar1=1.0, scalar2=None, op0=ALU.max),
                 reads=[RC.b[0]], writes=[RC.b[0]])
            P.op(P.dve, lambda e: e.reciprocal(out=RC[:, :], in_=RC[:, :]), reads=[RC.b[0]], writes=[RC.b[0]])
        P.op(P.pool, lambda e: e.tensor_tensor(out=HM[:, :], in0=k.H[:, c, :], in1=VM[:, :], op=ALU.mult),
             reads=[k.H.b[c], VM.b[0]], writes=[HM.b[0]])
        wsum(P.pool if c % 2 else P.dve, WS, HM, widx)
        P.op(P.dve, lambda e: e.tensor_tensor(out=WS[:, :], in0=WS[:, :], in1=RC[:, :], op=ALU.mult),
             reads=[WS.b[0], RC.b[0]], writes=[WS.b[0]])
        P.op(P.dve, lambda e: e.tensor_tensor(out=k.H[:, c, :], in0=WS[:, :], in1=k.H[:, c, :], op=ALU.subtract),
             reads=[WS.b[0], k.H.b[c]], writes=[k.H.b[c]])
    for cls in range(2):
        P.op(P.dve, lambda e: e.tensor_tensor(out=PSC[:, cls, :], in0=k.DER[:, li, cls, 2, :], in1=pvs(k, "pool_scale"), op=ALU.mult),
             reads=[k.DER.b[0], k.PV.b[0]], writes=[PSC.b[0]])
    for g0 in (0, 2):
        s, sb = W.get("pool")
        for gi in range(2):
            g = g0 + gi
            for jo in range(4):
                d = g * 4 + jo
                for (lo, hi) in k.tiles:
                    n = hi - lo
                    ps = psum(k)
                    for kk in range(4):
                        off = gi * 2048 + kk * 512 + jo * 128
                        P.op(P.pe, lambda e, kk=kk, off=off: e.matmul(
                            ps[:, :n], W.ring[:, s, off:off + 128], k.H[:, g * 4 + kk, lo:hi], start=(kk == 0), stop=(kk == 3)),
                            reads=[sb, k.H.b[g * 4 + kk]], writes=[ps.b[0]], signal=(kk == 3))
                    emit_resid(P, k, ps, d, lo, hi, lambda cls, d: PSC[:, cls, d:d + 1], extra_reads=[PSC.b[0]])
        W.release((s, sb))
    ph.close()


SCALE = HD ** -0.5
LAM_INIT3 = 0.8 - 0.6 * math.exp(-0.3 * 3)
NKT = 66
KL = 8192


def emit_outproj(P, k, W, SRC, scal_fn, extra_reads=()):
    for d2 in range(0, NCH, 2):
        s, sb = W.get("proj")
        for which in range(2):
            d = d2 + which
            for (lo, hi) in k.tiles:
                n = hi - lo
                ps = psum(k)
                for cc in range(NCH):
                    P.op(P.pe, lambda e, cc=cc: e.matmul(
                        ps[:, :n], W.ring[:, s, cc * 256 + which * 128:cc * 256 + which * 128 + 128], SRC[:, cc, lo:hi],
                        start=(cc == 0), stop=(cc == NCH - 1)),
                        reads=[sb, SRC.b[cc]], writes=[ps.b[0]], signal=(cc == NCH - 1))
                emit_resid(P, k, ps, d, lo, hi, scal_fn, extra_reads=extra_reads)
        W.release((s, sb))


def emit_qkv(P, k, W, layer, qd, kd, vd, ropec, ropes):
    T, nl = k.T, k.nl
    gqa = layer == 2
    nq = 16
    nk = 4 if gqa else 16
    ph = Phase(P)
    RC_ = ph.tt("ROPEC", [128, nl], F32)
    RS_ = ph.tt("ROPES", [128, nl], F32)
    ldr = Buf("ldrope")
    P.dma(P.sp, RC_[:, :], ropec, writes=[RC_.b[0]], dbuf=ldr)
    P.dma(P.sp, RS_[:, :], ropes, writes=[RS_.b[0]], dbuf=ldr)
    P.fix([RC_.b[0], RS_.b[0]], ldr)
    QN = ph.tt("QN", [128, 2, 512], F32, nbuf=2)
    T1 = ph.tt("T1", [128, 2, 512], F32, nbuf=2)
    T2 = ph.tt("T2", [128, 2, 512], F32, nbuf=2)
    QK = ph.tt("QK", [128, 2, T], BF16, nbuf=2)
    VO = ph.tt("VO", [128, 2, 512], BF16, nbuf=2)
    GS = ph.tt("GS", [128, 2], F32)
    ones = k.CST[:, C_ONES:C_ONES + 128]
    rot = k.CST[:, C_ROT:C_ROT + 128]
    if gqa:
        P.op(P.dve, lambda e: e.tensor_scalar(out=GS[:, 0:1], in0=pvs(k, "qk_norm")[:, 0:1], scalar1=SCALE, scalar2=None, op0=ALU.mult),
             reads=[k.PV.b[0]], writes=[GS.b[0]])
        P.op(P.dve, lambda e: e.tensor_copy(out=GS[:, 1:2], in_=pvs(k, "qk_norm")[:, 1:2]),
             reads=[k.PV.b[0]], writes=[GS.b[0]])
    stq = [Buf("st_qk0"), Buf("st_qk1")]
    stv = [Buf("st_v0"), Buf("st_v1")]
    nmaps = nq + nk
    NT = len(k.tiles)
    items = [dict(m=m, ti=ti, lo=lo, hi=hi, idx=m * NT + ti) for m in range(nmaps) for ti, (lo, hi) in enumerate(k.tiles)]
    slots = {}

    def stageA(it):
        m, lo, hi = it["m"], it["lo"], it["hi"]
        n = hi - lo
        if m % 2 == 0 and it["ti"] == 0:
            slots[m // 2] = W.get("proj")
        s, sb = slots[m // 2]
        which = m % 2
        ps = psum(k)
        for cc in range(NCH):
            P.op(P.pe, lambda e, cc=cc: e.matmul(
                ps[:, :n], W.ring[:, s, cc * 256 + which * 128:cc * 256 + which * 128 + 128], k.H[:, cc, lo:hi],
                start=(cc == 0), stop=(cc == NCH - 1)),
                reads=[sb, k.H.b[cc]], writes=[ps.b[0]], signal=(cc == NCH - 1))
        it["ps"] = ps
        if gqa:
            sq, sqb = scr(k)
            P.op(P.act, lambda e: e.activation(out=sq[:, :n], in_=ps[:, :n], func=AF.Square), reads=[ps.b[0]], writes=[sqb])
            it["sq"], it["sqb"] = sq, sqb
        if m % 2 == 1 and it["ti"] == NT - 1:
            W.release(slots[m // 2])

    def stageB(it):
        m, lo, hi = it["m"], it["lo"], it["hi"]
        n = hi - lo
        isq = m < nq
        ps = it["ps"]
        qi = it["idx"] % 2
        qn, qnb = QN[:, qi, :], QN.b[qi]
        if gqa:
            sq, sqb = it["sq"], it["sqb"]
            pss = psum(k)
            P.op(P.pe, lambda e: e.matmul(pss[:, :n], ones, sq[:, :n], start=True, stop=True),
                 reads=[sqb, k.CST.b[0]], writes=[pss.b[0]])
            P.op(P.act, lambda e: e.activation(out=sq[:, :n], in_=pss[:, :n], func=AF.Sqrt, bias=k.EPS[:, 1:2], scale=1.0 / HD),
                 reads=[pss.b[0], k.EPS.b[0]], writes=[sqb])
            P.op(P.dve, lambda e: e.reciprocal(out=sq[:, :n], in_=sq[:, :n]), reads=[sqb], writes=[sqb])
            gcol = GS[:, 0:1] if isq else GS[:, 1:2]
            P.op(P.dve, lambda e: e.scalar_tensor_tensor(out=qn[:, :n], in0=ps[:, :n], scalar=gcol, in1=sq[:, :n],
                                                         op0=ALU.mult, op1=ALU.mult),
                 reads=[ps.b[0], sqb, GS.b[0]], writes=[qnb])
        else:
            P.op(P.act, lambda e: e.mul(out=qn[:, :n], in_=ps[:, :n], mul=(SCALE if isq else 1.0)),
                 reads=[ps.b[0]], writes=[qnb])

    def stageC(it):
        m, lo, hi = it["m"], it["lo"], it["hi"]
        isq = m < nq
        qi = it["idx"] % 2
        z = m % 2
        qn, qnb = QN[:, qi, :], QN.b[qi]
        for (a, b_, cls) in cls_split(lo, hi, nl):
            w_ = b_ - a
            o0 = a - lo
            if cls == 0:
                pr = psum(k)
                P.op(P.pe, lambda e: e.matmul(pr[:, :w_], rot, qn[:, o0:o0 + w_], start=True, stop=True),
                     reads=[qnb, k.CST.b[0]], writes=[pr.b[0]])
                P.op(P.pool, lambda e: e.tensor_tensor(out=T1[:, qi, :w_], in0=qn[:, o0:o0 + w_], in1=RC_[:, a:b_], op=ALU.mult),
                     reads=[qnb, RC_.b[0]], writes=[T1.b[qi]])
                P.op(P.dve, lambda e: e.tensor_tensor(out=T2[:, qi, :w_], in0=pr[:, :w_], in1=RS_[:, a:b_], op=ALU.mult),
                     reads=[pr.b[0], RS_.b[0]], writes=[T2.b[qi]])
                P.op(P.pool, lambda e: e.tensor_tensor(out=QK[:, z, a:b_], in0=T1[:, qi, :w_], in1=T2[:, qi, :w_], op=ALU.add),
                     reads=[T1.b[qi], T2.b[qi]], writes=[QK.b[z]])
            else:
                P.op(P.act, lambda e: e.copy(out=QK[:, z, a:b_], in_=qn[:, o0:o0 + w_]),
                     reads=[qnb], writes=[QK.b[z]])
        if it["ti"] == NT - 1:
            dst = qd[m * 128:(m + 1) * 128, :] if isq else kd[(m - nq) * 128:(m - nq + 1) * 128, :]
            P.dma(P.sp, dst, QK[:, z, 0:T], reads=[QK.b[z]], dbuf=stq[z])

    NI = len(items)
    for idx in range(NI + 2):
        if idx < NI:
            stageA(items[idx])
        if 0 <= idx - 1 < NI:
            stageB(items[idx - 1])
        if 0 <= idx - 2 < NI:
            stageC(items[idx - 2])
    ngrp = 1 if gqa else 4
    nt128 = (T + 127) // 128
    vcnt = 0
    for gi in range(ngrp):
        sl = [W.get("vproj"), W.get("vproj")]
        for t in range(nt128):
            lo, hi = t * 128, min(T, (t + 1) * 128)
            n = hi - lo
            ps = psum(k)
            for cc in range(NCH):
                s, sb = sl[cc // 8]
                P.op(P.pe, lambda e, cc=cc, s=s: e.matmul(
                    ps[:n, :], k.H[:, cc, lo:hi], W.ring[:, s, (cc % 8) * 512:(cc % 8 + 1) * 512],
                    start=(cc == 0), stop=(cc == NCH - 1)),
                    reads=[sb, k.H.b[cc]], writes=[ps.b[0]], signal=(cc == NCH - 1))
            vi = vcnt % 2
            vcnt += 1
            eng = P.act if vi == 0 else P.dve
            if vi == 0:
                P.op(P.act, lambda e: e.copy(out=VO[:n, vi, :], in_=ps[:n, :]), reads=[ps.b[0]], writes=[VO.b[vi]])
            else:
                P.op(P.dve, lambda e: e.tensor_copy(out=VO[:n, vi, :], in_=ps[:n, :]), reads=[ps.b[0]], writes=[VO.b[vi]])
            P.dma(P.sp, vd[lo:hi, gi * 512:(gi + 1) * 512], VO[:n, vi, :], reads=[VO.b[vi]], dbuf=stv[vi])
        for x_ in sl:
            W.release(x_)
    k.kv_store = stq + stv
    ph.close()


NCHK = 9


def kchunk(kt):
    return kt // 8 if kt < 64 else 8


def load_kv(P, k, KT, VT, k_all, v_all, maps, nk, vcol0, vw, src=()):
    T = TB
    kv = k_all.rearrange("(r m p) t -> p r m t", r=NCORE, m=nk)
    for r in range(NCORE):
        for mi, m in enumerate(maps):
            P.dma(P.sp, KT[:, mi, r * LAT_PC:(r + 1) * LAT_PC], kv[:, r, m, 0:LAT_PC],
                  reads=list(src), writes=[KT.b[r]], dbuf=KT.b[r])
        P.dma(P.sp, VT[:, r * 8:(r + 1) * 8, :],
              v_all[r * T:r * T + LAT_PC, vcol0:vcol0 + vw].rearrange("(j p) n -> p j n", p=128),
              reads=list(src), writes=[VT.b[r]], dbuf=VT.b[r])
    for mi, m in enumerate(maps):
        with P.nc.allow_non_contiguous_dma(reason="ctx keys"):
            P.dma(P.sp, KT[:, mi, KL:KL + CTX].rearrange("p (r t) -> p r t", r=NCORE), kv[:, :, m, LAT_PC:T],
                  reads=list(src), writes=[KT.b[8]], dbuf=KT.b[8])
    for r in range(NCORE):
        P.dma(P.sp, VT[(r % 4) * 32:(r % 4 + 1) * 32, 64 + r // 4, :],
              v_all[r * T + LAT_PC:(r + 1) * T, vcol0:vcol0 + vw],
              reads=list(src), writes=[VT.b[8]], dbuf=VT.b[8])


LOOKAHEAD = 3


SUM_ROLES = ("dve", "pool", "dve", "pe")
SUM_TAIL = 6


def emit_attn_core(P, k, QTm, qcols, KT, mi, VT, vw, PT, pti, kts, ACC, ACCR):
    a, b_ = qcols
    n = b_ - a
    nvo = vw // 128
    npt = len(PT.b)
    pos = [ps_reserve(k) for _ in range(nvo)]
    psm = ps_reserve(k)
    pend = []
    nk_ = len(kts)
    nacc = max(0, nk_ - SUM_TAIL)
    if nacc < 4:
        nacc = 0
    roles = [SUM_ROLES[ii % 4] if ii < nacc else "pe" for ii in range(nk_)]
    first_of = {r: roles.index(r) for r in set(roles)}
    st = {"pe_started": False}

    def flush():
        ii, kt, pi = pend.pop(0)
        first, last = ii == 0, ii == nk_ - 1
        ch = kchunk(kt)
        role = roles[ii]
        for j in range(nvo):
            P.op(P.pe, lambda e, j=j: e.matmul(pos[j][:, :n], VT[:, kt, j * 128:(j + 1) * 128], PT[:, pi, :n], start=first, stop=last),
                 reads=[VT.b[ch], PT.b[pi]], writes=[pos[j].b[0]], signal=(last or (j == nvo - 1 and role != "pe")))
        if role == "pe":
            P.op(P.pe, lambda e: e.matmul(psm[:, :n], k.ONEB[:, :], PT[:, pi, :n], start=not st["pe_started"], stop=(last and nacc == 0)),
                 reads=[k.ONEB.b[0], PT.b[pi]], writes=[psm.b[0]], signal=True)
            st["pe_started"] = True
        else:
            E = P.dve if role == "dve" else P.pool
            ai = 0 if role == "dve" else 1
            acc, accb = ACC[:, ai, :n], ACC.b[ai]
            if ii == first_of[role]:
                P.op(E, lambda e: e.tensor_copy(out=acc, in_=PT[:, pi, :n]), reads=[PT.b[pi]], writes=[accb])
            else:
                P.op(E, lambda e: e.tensor_tensor(out=acc, in0=acc, in1=PT[:, pi, :n], op=ALU.add),
                     reads=[PT.b[pi], accb], writes=[accb])
        if nacc and ii == nacc - 1:
            P.op(P.dve, lambda e: e.tensor_tensor(out=ACCR[:, 0, :n].bitcast(F32R), in0=ACC[:, 0, :n], in1=ACC[:, 1, :n], op=ALU.add),
                 reads=[ACC.b[0], ACC.b[1]], writes=[ACCR.b[0]])

    for ii, kt in enumerate(kts):
        ps = psum(k)
        ch = kchunk(kt)
        P.op(P.pe, lambda e: e.matmul(ps[:, :n], KT[:, mi, kt * 128:(kt + 1) * 128], QTm(a, b_), start=True, stop=True),
             reads=[KT.b[ch], k.QT.b[0]], writes=[ps.b[0]])
        pi = pti[0] % npt
        pti[0] += 1
        P.op(P.act, lambda e: e.activation(out=PT[:, pi, :n], in_=ps[:, :n], func=AF.Exp),
             reads=[ps.b[0]], writes=[PT.b[pi]])
        pend.append((ii, kt, pi))
        if len(pend) > LOOKAHEAD:
            flush()
    while pend:
        flush()
    if nacc:
        P.op(P.pe, lambda e: e.matmul(psm[:, :n], k.ONER[:, :].bitcast(F32R), ACCR[:, 0, :n].bitcast(F32R),
                                      start=not st["pe_started"], stop=True),
             reads=[k.ONER.b[0], ACCR.b[0]], writes=[psm.b[0]], signal=True)
    return pos, psm


def emit_attn_gqa(P, k, q_d, k_all, v_all, src=()):
    ph = Phase(P)
    T = k.T
    KT = ph.tt("KT", [128, 1, KL + CTX], BF16, nbuf=NCHK)
    VT = ph.tt("VT", [128, NKT, 128], BF16, nbuf=NCHK)
    k.QT = ph.tt("QT", [128, 4, T], BF16)
    PT = ph.tt("PT", [128, 5, 512], BF16, nbuf=5)
    REC = ph.tt("REC", [128, 512], F32)
    ACC = ph.tt("ACC", [128, 2, 512], F32, nbuf=2)
    ACCR = ph.tt("ACCR", [128, 1, 512], F32)
    pti = [0]
    for g in range(4):
        load_kv(P, k, KT, VT, k_all, v_all, [g], 4, g * 128, 128, src)
        P.dma(P.sp, k.QT[:, :, :], q_d[g * 512:(g + 1) * 512, :].rearrange("(h p) t -> p h t", p=128),
              writes=[k.QT.b[0]], dbuf=k.QT.b[0])
        for hh in range(4):
            hq = g * 4 + hh
            work = [((0, 512), list(range(NKT))), ((512, 1024), list(range(NKT))), ((1024, T), [64, 65])]
            for (a, b_), kts in work:
                n = b_ - a
                pos, psm = emit_attn_core(P, k, lambda x, y: k.QT[:, hh, x:y], (a, b_), KT, 0, VT, 128, PT, pti, kts, ACC, ACCR)
                P.op(P.dve, lambda e: e.reciprocal(out=REC[:, :n], in_=psm[:, :n]), reads=[psm.b[0]], writes=[REC.b[0]])
                P.op(P.dve, lambda e: e.tensor_tensor(out=k.H[:, hq, a:b_], in0=pos[0][:, :n], in1=REC[:, :n], op=ALU.mult),
                     reads=[pos[0].b[0], REC.b[0]], writes=[k.H.b[hq]])
                for t_ in pos + [psm]:
                    ps_unreserve(k, t_)
    ph.close()


def emit_attn_diff(P, k, q_d, k_all, v_all, src=()):
    ph = Phase(P)
    T = k.T
    KT = ph.tt("KT", [128, 2, KL + CTX], BF16, nbuf=NCHK)
    VT = ph.tt("VT", [128, NKT, 256], BF16, nbuf=NCHK)
    k.QT = ph.tt("QT", [128, 2, LAT_PC], BF16)
    PT = ph.tt("PT", [128, 5, 512], BF16, nbuf=5)
    REC = ph.tt("REC", [128, 512], F32)
    ACC = ph.tt("ACC", [128, 2, 512], F32, nbuf=2)
    ACCR = ph.tt("ACCR", [128, 1, 512], F32)
    O0 = ph.tt("O0", [128, 2, 512], F32)
    OO = ph.tt("OO", [128, 2, 512], F32)
    LAM = ph.tt("LAM", [128, 8], F32)
    ones = k.CST[:, C_ONES:C_ONES + 128]
    lam = pvs(k, "lam")
    rw = dict(reads=[LAM.b[0], k.PV.b[0]], writes=[LAM.b[0]])
    P.op(P.dve, lambda e: e.tensor_tensor(out=LAM[:, 0:1], in0=lam[:, 0:1], in1=lam[:, 1:2], op=ALU.mult), **rw)
    P.op(P.dve, lambda e: e.tensor_tensor(out=LAM[:, 1:2], in0=lam[:, 2:3], in1=lam[:, 3:4], op=ALU.mult), **rw)
    psl = psum(k)
    P.op(P.pe, lambda e: e.matmul(psl[:, 0:2], ones, LAM[:, 0:2], start=True, stop=True),
         reads=[LAM.b[0], k.CST.b[0]], writes=[psl.b[0]])
    P.op(P.act, lambda e: e.activation(out=LAM[:, 2:4], in_=psl[:, 0:2], func=AF.Exp), reads=[psl.b[0]], writes=[LAM.b[0]])
    P.op(P.dve, lambda e: e.tensor_tensor(out=LAM[:, 4:5], in0=LAM[:, 3:4], in1=LAM[:, 2:3], op=ALU.subtract), **rw)
    P.op(P.dve, lambda e: e.tensor_scalar(out=LAM[:, 4:5], in0=LAM[:, 4:5], scalar1=-LAM_INIT3, scalar2=None, op0=ALU.add), **rw)
    P.op(P.dve, lambda e: e.tensor_scalar(out=LAM[:, 5:7], in0=pvs(k, "subln"), scalar1=1.0 - LAM_INIT3, scalar2=None, op0=ALU.mult), **rw)
    nlam = LAM[:, 4:5]
    pti = [0]
    for h in range(8):
        load_kv(P, k, KT, VT, k_all, v_all, [2 * h, 2 * h + 1], 16, h * 256, 256, src)
        P.dma(P.sp, k.QT[:, :, :], q_d[h * 256:(h + 1) * 256, 0:LAT_PC].rearrange("(m p) t -> p m t", p=128),
              writes=[k.QT.b[0]], dbuf=k.QT.b[0])
        for (a, b_) in ((0, 512), (512, 1024)):
            n = b_ - a
            for mp in range(2):
                pos, psm = emit_attn_core(P, k, lambda x, y: k.QT[:, mp, x:y], (a, b_), KT, mp, VT, 256, PT, pti, list(range(NKT)), ACC, ACCR)
                P.op(P.dve, lambda e: e.reciprocal(out=REC[:, :n], in_=psm[:, :n]), reads=[psm.b[0]], writes=[REC.b[0]])
                for j in range(2):
                    if mp == 0:
                        P.op(P.dve, lambda e, j=j: e.tensor_tensor(out=O0[:, j, :n], in0=pos[j][:, :n], in1=REC[:, :n], op=ALU.mult),
                             reads=[pos[j].b[0], REC.b[0]], writes=[O0.b[0]])
                    else:
                        P.op(P.dve, lambda e, j=j: e.tensor_tensor(out=OO[:, j, :n], in0=pos[j][:, :n], in1=REC[:, :n], op=ALU.mult),
                             reads=[pos[j].b[0], REC.b[0]], writes=[OO.b[0]])
                        P.op(P.dve, lambda e, j=j: e.scalar_tensor_tensor(out=OO[:, j, :n], in0=OO[:, j, :n], scalar=nlam, in1=O0[:, j, :n],
                                                                          op0=ALU.mult, op1=ALU.add),
                             reads=[OO.b[0], O0.b[0], LAM.b[0]], writes=[OO.b[0]])
                for t_ in pos + [psm]:
                    ps_unreserve(k, t_)
            pss = psum(k)
            for j in range(2):
                sq, sqb = scr(k)
                P.op(P.act, lambda e, j=j: e.activation(out=sq[:, :n], in_=OO[:, j, :n], func=AF.Square), reads=[OO.b[0]], writes=[sqb])
                P.op(P.pe, lambda e, j=j: e.matmul(pss[:, :n], ones, sq[:, :n], start=(j == 0), stop=(j == 1)),
                     reads=[sqb, k.CST.b[0]], writes=[pss.b[0]])
            sq, sqb = scr(k)
            P.op(P.act, lambda e: e.activation(out=sq[:, :n], in_=pss[:, :n], func=AF.Sqrt, bias=k.EPS[:, 1:2], scale=1.0 / 256),
                 reads=[pss.b[0], k.EPS.b[0]], writes=[sqb])
            P.op(P.dve, lambda e: e.reciprocal(out=sq[:, :n], in_=sq[:, :n]), reads=[sqb], writes=[sqb])
            for j in range(2):
                P.op(P.dve, lambda e, j=j: e.scalar_tensor_tensor(out=k.H[:, 2 * h + j, a:b_], in0=OO[:, j, :n], scalar=LAM[:, 5 + j:6 + j],
                                                                  in1=sq[:, :n], op0=ALU.mult, op1=ALU.mult),
                     reads=[OO.b[0], sqb, LAM.b[0]], writes=[k.H.b[2 * h + j]])
    for c in range(NCH):
        P.op(P.pool, lambda e, c=c: e.memset(k.H[:, c, LAT_PC:T], 0.0), writes=[k.H.b[c]])
    ph.close()


def plan_qkv(layer):
    p = []
    if layer == 2:
        name, nm, vcol, ng = "gqa_qkv_w", 20, 2560, 1
    else:
        name, nm, vcol, ng = "diff_qkv_w", 32, 4096, 4
    for m in range(0, nm, 2):
        p.append(("proj", name, 0, [m * 128, (m + 1) * 128]))
    for gi in range(ng):
        p.append(("vproj", name, 0, 0, vcol + gi * 512))
        p.append(("vproj", name, 0, 8, vcol + gi * 512))
    return p


def plan_outproj(name):
    return [("proj", name, 0, [d * 128, (d + 1) * 128]) for d in range(0, 16, 2)]


def plan_launch(which, stop_after=None):
    if which == "A":
        assert stop_after is None
        return plan_mod(0) + plan_layer0() + plan_moe(0, side=1) + plan_layer1() + plan_moe(1, side=2) + plan_qkv(2)
    if which == "B":
        return [("fence",)] + plan_outproj("gqa_out_w") + plan_moe(2, side=3) + plan_qkv(3)
    if which == "C":
        return [("fence",)] + plan_outproj("diff_out_w") + plan_moe(3)
    raise ValueError(which)


BF = mybir.dt.bfloat16


def build_launch(which, pv, plan, stop_after=None):
    nc = bass.Bass("TRN2", target_bir_lowering=False)
    P = Prog(nc)
    TX = TA if which == "A" else TB
    nslots = len([sp for sp in plan if sp[0] != "fence"])
    xin = nc.dram_tensor("xin", [128, NCH, TX], F32, kind="ExternalInput").ap()
    pvd = nc.dram_tensor("pvec", [128, pv.n], F32, kind="ExternalInput").ap()
    cstd = nc.dram_tensor("cst", [128, 384], F32, kind="ExternalInput").ap()
    wd = nc.dram_tensor("wstream", [nslots, 128, SLOT], F32, kind="ExternalInput").ap()
    xout = nc.dram_tensor("xout", [128, NCH, TB], F32, kind="ExternalOutput").ap()
    layers = {"A": [0, 1, 2], "B": [2, 3], "C": [3]}[which]
    full = stop_after is None
    if which in ("A", "B") and full:
        nk_o = 4 if which == "A" else 16
        nv_o = 512 if which == "A" else 2048
        ropec = nc.dram_tensor("ropec", [128, LAT_PC], F32, kind="ExternalInput").ap()
        ropes = nc.dram_tensor("ropes", [128, LAT_PC], F32, kind="ExternalInput").ap()
        qd_o = nc.dram_tensor("qd_o", [16 * 128, TB], BF, kind="ExternalOutput").ap()
        kd_o = nc.dram_tensor("kd_o", [nk_o * 128, TB], BF, kind="ExternalOutput").ap()
        vd_o = nc.dram_tensor("vd_o", [TB, nv_o], BF, kind="ExternalOutput").ap()
    if which in ("B", "C"):
        nk_i = 4 if which == "B" else 16
        nv_i = 512 if which == "B" else 2048
        qd_i = nc.dram_tensor("qd_i", [16 * 128, TB], BF, kind="ExternalInput").ap()
        k_all = nc.dram_tensor("k_all", [NCORE * nk_i * 128, TB], BF, kind="ExternalInput").ap()
        v_all = nc.dram_tensor("v_all", [NCORE * TB, nv_i], BF, kind="ExternalInput").ap()
    k = K()
    setup_common(P, k, TX, NLA if which == "A" else LAT_PC, pv, layers, pvd, cstd)
    ld = Buf("ldx")
    if which == "A":
        vmd = nc.dram_tensor("vm", [128, TA], F32, kind="ExternalInput").ap()
        k.VM = TT(P, "VM", [128, TA], F32)
        P.dma(P.sp, k.VM[:, :], vmd, writes=[k.VM.b[0]], dbuf=ld)
    for c4 in range(0, NCH, 4):
        P.dma(P.sp, k.X[:, c4:c4 + 4, :], xin[:, c4:c4 + 4, :], writes=k.X.b[c4:c4 + 4], dbuf=ld)
    P.fix(([k.VM.b[0]] if which == "A" else []) + k.X.b, ld)
    W = WStream(P, plan, wd)
    emit_silu_c(P, k)
    if which == "A":
        emit_mod(P, k, W, 0)
    else:
        modst_i = nc.dram_tensor("modst_i", [128, 384], F32, kind="ExternalInput").ap()
        li0 = k.lidx[layers[0]]
        P.dma(P.sp, k.MOD[:, li0, :, :], modst_i[:, 0:192].rearrange("p (a b) -> p a b", a=2),
              writes=[k.MOD.b[0]], dbuf=ld)
        P.dma(P.sp, k.DER[:, li0, :, :, :], modst_i[:, 192:384].rearrange("p (a b c) -> p a b c", a=2, b=6),
              writes=[k.DER.b[0]], dbuf=ld)
        P.fix([k.MOD.b[0], k.DER.b[0]] + k.X.b, ld)
    if which in ("A", "B"):
        modst_o = nc.dram_tensor("modst_o", [128, 384], F32, kind="ExternalOutput").ap()

    def tail(layer, side=None):
        emit_ln(P, k, layer, 0, True)
        emit_moe(P, k, W, layer, side=side)
        emit_ln(P, k, layer, 1, False)

    if which == "A":
        emit_modulate(P, k, 0)
        emit_conv_mixer(P, k, W, 0)
        tail(0, side=mod_steps(P, k, W, 1))
        emit_modulate(P, k, 1)
        emit_pool_mixer(P, k, W, 1)
        tail(1, side=mod_steps(P, k, W, 2))
        ph = Phase(P)
        TMP = ph.tt("TMP", [128, 2, LAT_PC], F32, nbuf=2)
        for c in range(NCH):
            z = c % 2
            e1, e2 = (P.dve, P.pool) if z == 0 else (P.pool, P.dve)
            P.op(e1, lambda e, c=c, z=z: e.tensor_copy(out=TMP[:, z, :], in_=k.X[:, c, HALO:HALO + LAT_PC]),
                 reads=[k.X.b[c]], writes=[TMP.b[z]])
            P.op(P.act, lambda e, c=c: e.copy(out=k.X[:, c, LAT_PC:TB], in_=k.X[:, c, NLA + HALO:NLA + HALO + CTX_PC]),
                 reads=[k.X.b[c]], writes=[k.X.b[c]])
            P.op(e2, lambda e, c=c, z=z: e.tensor_copy(out=k.X[:, c, 0:LAT_PC], in_=TMP[:, z, :]),
                 reads=[TMP.b[z], k.X.b[c]], writes=[k.X.b[c]])
        ph.close()
        k.T, k.nl = TB, LAT_PC
        k.tiles = tok_tiles(TB)
        if full:
            emit_modulate(P, k, 2)
            emit_qkv(P, k, W, 2, qd_o, kd_o, vd_o, ropec, ropes)
    elif which == "B":
        W.suspend()
        emit_attn_gqa(P, k, qd_i, k_all, v_all)
        W.resume()
        li = k.lidx[2]
        emit_outproj(P, k, W, k.H, lambda cls, d: k.DER[:, li, cls, 2, d:d + 1])
        tail(2, side=mod_steps(P, k, W, 3))
        emit_modulate(P, k, 3)
        emit_qkv(P, k, W, 3, qd_o, kd_o, vd_o, ropec, ropes)
    else:
        W.suspend()
        emit_attn_diff(P, k, qd_i, k_all, v_all)
        W.resume()
        li = k.lidx[3]
        emit_outproj(P, k, W, k.H, lambda cls, d: k.DER[:, li, cls, 2, d:d + 1])
        tail(3)
    st = Buf("st")
    for c4 in range(0, NCH, 4):
        P.dma(P.sp, xout[:, c4:c4 + 4, :], k.X[:, c4:c4 + 4, 0:TB], reads=k.X.b[c4:c4 + 4], dbuf=st)
    if which in ("A", "B"):
        lo_ = k.lidx[layers[-1]]
        P.dma(P.sp, modst_o[:, 0:192].rearrange("p (a b) -> p a b", a=2), k.MOD[:, lo_, :, :],
              reads=[k.MOD.b[0]], dbuf=st)
        P.dma(P.sp, modst_o[:, 192:384].rearrange("p (a b c) -> p a b c", a=2, b=6), k.DER[:, lo_, :, :, :],
              reads=[k.DER.b[0]], dbuf=st)
    P.sp.e.wait_ge(st.dsem, st.dcnt)
    if hasattr(k, "kv_store"):
        for b_ in k.kv_store:
            if b_.dsem is not None:
                P.sp.e.wait_ge(b_.dsem, b_.dcnt)
    assert P.pe.last_signaled
    assert W.cur == len(plan), (W.cur, len(plan))
    return nc, P


def plan_fused():
    return (plan_mod(0) + plan_layer0() + plan_moe(0, side=1) + plan_layer1() + plan_moe(1, side=2)
            + plan_qkv(2) + [("fence",)] + plan_outproj("gqa_out_w") + plan_moe(2, side=3)
            + plan_qkv(3) + [("fence",)] + plan_outproj("diff_out_w") + plan_moe(3))


def emit_gather(P, src, dst, name):
    b = Buf(name)
    b.dsem = P.nc.alloc_semaphore("csem_" + name)
    P.sems.append(b.dsem)
    ins = P.pool.e.collective_compute("AllGather", ALU.bypass, replica_groups=[list(range(NCORE))], ins=[src], outs=[dst])
    ins.then_inc(b.dsem, 16)
    b.dcnt = 16
    b.w = (b.dsem, 16)
    return b


def build_fused(pv, plan):
    nc = bass.Bass("TRN2", target_bir_lowering=False)
    P = Prog(nc)
    nslots = len([sp for sp in plan if sp[0] != "fence"])
    xin = nc.dram_tensor("xin", [128, NCH, TA], F32, kind="ExternalInput").ap()
    vmd = nc.dram_tensor("vm", [128, TA], F32, kind="ExternalInput").ap()
    pvd = nc.dram_tensor("pvec", [128, pv.n], F32, kind="ExternalInput").ap()
    cstd = nc.dram_tensor("cst", [128, 384], F32, kind="ExternalInput").ap()
    wd = nc.dram_tensor("wstream", [nslots, 128, SLOT], F32, kind="ExternalInput").ap()
    ropec = nc.dram_tensor("ropec", [128, LAT_PC], F32, kind="ExternalInput").ap()
    ropes = nc.dram_tensor("ropes", [128, LAT_PC], F32, kind="ExternalInput").ap()
    xout = nc.dram_tensor("xout", [128, NCH, LAT_PC], F32, kind="ExternalOutput").ap()
    dr = {}
    for l, nk_, nv_ in ((2, 4, 512), (3, 16, 2048)):
        dr[l] = dict(
            q=nc.dram_tensor("qd%d" % l, [16 * 128, TB], BF, kind="Internal").ap(),
            k=nc.dram_tensor("kd%d" % l, [nk_ * 128, TB], BF, kind="Internal").ap(),
            v=nc.dram_tensor("vd%d" % l, [TB, nv_], BF, kind="Internal").ap(),
            ka=nc.dram_tensor("kall%d" % l, [NCORE * nk_ * 128, TB], BF, kind="Internal").ap(),
            va=nc.dram_tensor("vall%d" % l, [NCORE * TB, nv_], BF, kind="Internal").ap())
    layers = [0, 1, 2, 3]
    k = K()
    setup_common(P, k, TA, NLA, pv, layers, pvd, cstd)
    ld = Buf("ldx")
    k.VM = TT(P, "VM", [128, TA], F32)
    P.dma(P.sp, k.VM[:, :], vmd, writes=[k.VM.b[0]], dbuf=ld)
    for c4 in range(0, NCH, 4):
        P.dma(P.sp, k.X[:, c4:c4 + 4, :], xin[:, c4:c4 + 4, :], writes=k.X.b[c4:c4 + 4], dbuf=ld)
    P.fix([k.VM.b[0]] + k.X.b, ld)
    W = WStream(P, plan, wd)
    emit_silu_c(P, k)
    emit_mod(P, k, W, 0)
    emit_modulate(P, k, 0)
    emit_conv_mixer(P, k, W, 0)
    emit_ln(P, k, 0, 0, True)
    emit_moe(P, k, W, 0, side=mod_steps(P, k, W, 1))
    emit_ln(P, k, 0, 1, False)
    emit_modulate(P, k, 1)
    emit_pool_mixer(P, k, W, 1)
    emit_ln(P, k, 1, 0, True)
    emit_moe(P, k, W, 1, side=mod_steps(P, k, W, 2))
    emit_ln(P, k, 1, 1, False)
    ph = Phase(P)
    TMP = ph.tt("TMP", [128, 2, LAT_PC], F32, nbuf=2)
    for c in range(NCH):
        z = c % 2
        e1, e2 = (P.dve, P.pool) if z == 0 else (P.pool, P.dve)
        P.op(e1, lambda e, c=c, z=z: e.tensor_copy(out=TMP[:, z, :], in_=k.X[:, c, HALO:HALO + LAT_PC]),
             reads=[k.X.b[c]], writes=[TMP.b[z]])
        P.op(P.act, lambda e, c=c: e.copy(out=k.X[:, c, LAT_PC:TB], in_=k.X[:, c, NLA + HALO:NLA + HALO + CTX_PC]),
             reads=[k.X.b[c]], writes=[k.X.b[c]])
        P.op(e2, lambda e, c=c, z=z: e.tensor_copy(out=k.X[:, c, 0:LAT_PC], in_=TMP[:, z, :]),
             reads=[TMP.b[z], k.X.b[c]], writes=[k.X.b[c]])
    ph.close()
    k.T, k.nl = TB, LAT_PC
    k.tiles = tok_tiles(TB)
    for l in (2, 3):
        d = dr[l]
        emit_modulate(P, k, l)
        emit_qkv(P, k, W, l, d["q"], d["k"], d["v"], ropec, ropes)
        gk = emit_gather(P, d["k"], d["ka"], "k%d" % l)
        gv = emit_gather(P, d["v"], d["va"], "v%d" % l)
        W.suspend()
        if l == 2:
            emit_attn_gqa(P, k, d["q"], d["ka"], d["va"], src=[gk, gv])
        else:
            emit_attn_diff(P, k, d["q"], d["ka"], d["va"], src=[gk, gv])
        W.resume()
        li = k.lidx[l]
        emit_outproj(P, k, W, k.H, lambda cls, dd, li=li: k.DER[:, li, cls, 2, dd:dd + 1])
        emit_ln(P, k, l, 0, True)
        emit_moe(P, k, W, l, side=(mod_steps(P, k, W, 3) if l == 2 else None))
        emit_ln(P, k, l, 1, False)
    st = Buf("st")
    for c4 in range(0, NCH, 4):
        P.dma(P.sp, xout[:, c4:c4 + 4, :], k.X[:, c4:c4 + 4, 0:LAT_PC], reads=k.X.b[c4:c4 + 4], dbuf=st)
    P.sp.e.wait_ge(st.dsem, st.dcnt)
    assert P.pe.last_signaled
    assert W.cur == len(plan), (W.cur, len(plan))
    return nc, P


def run_fused(inp, trace=False):
    plan = plan_fused()
    pv = make_pvec(inp, [0, 1, 2, 3])
    ws = stream_array(inp, plan)
    nc, P = build_fused(pv, plan)
    print("fused: instructions", P.ninst, "waits", P.nwait, "slots", ws.shape[0], flush=True)
    xs, vms = prep_A(inp)
    cst = make_consts()
    pva = pv.array()
    in_maps = []
    for r in range(NCORE):
        c_, s_ = rope_tables(r)
        in_maps.append({"xin": xs[r], "vm": vms[r], "pvec": pva, "cst": cst, "wstream": ws, "ropec": c_, "ropes": s_})
    res = run_bass_kernel_spmd(nc, in_maps, core_ids=list(range(NCORE)), trace=trace)
    if trace:
        print("exec_time_ns", res.exec_time_ns, flush=True)
    out = np.empty((1, SEQ, D), np.float32)
    for r in range(NCORE):
        o = res.results[r]["xout"]
        out[0, r * LAT_PC:(r + 1) * LAT_PC] = o.transpose(2, 1, 0).reshape(LAT_PC, D)
    return out


def prep_A(inp):
    x = inp["x"][0]
    ctx = inp["ctx"][0]
    xs, vms = [], []
    for r in range(NCORE):
        buf = np.zeros((TA, D), np.float32)
        vm = np.zeros((TA,), np.float32)
        lo = r * LAT_PC - HALO
        a, b = max(lo, 0), min(lo + NLA, SEQ)
        buf[a - lo:b - lo] = x[a:b]
        vm[a - lo:b - lo] = 1.0
        lo = r * CTX_PC - HALO
        a, b = max(lo, 0), min(lo + NCA, CTX)
        buf[NLA + a - lo:NLA + b - lo] = ctx[a:b]
        vm[NLA + a - lo:NLA + b - lo] = 1.0
        xs.append(np.ascontiguousarray(buf.reshape(TA, NCH, 128).transpose(2, 1, 0)))
        vms.append(np.ascontiguousarray(np.broadcast_to(vm[None, :], (128, TA))))
    return xs, vms


def rope_tables(r):
    pos = np.arange(r * LAT_PC, (r + 1) * LAT_PC)
    row = (pos // GRID_W).astype(np.float32)
    col = (pos % GRID_W).astype(np.float32)
    inv = (10000.0 ** (-np.arange(0, 64, 2, dtype=np.float32) / 64.0)).astype(np.float32)
    ang = np.concatenate([row[None, :] * inv[:, None], row[None, :] * inv[:, None],
                          col[None, :] * inv[:, None], col[None, :] * inv[:, None]], axis=0).astype(np.float32)
    return np.ascontiguousarray(np.cos(ang).astype(np.float32)), np.ascontiguousarray(np.sin(ang).astype(np.float32))


def run_launch(which, inp, state, stop_after=None, trace=False):
    plan = plan_launch(which, stop_after)
    layers = {"A": [0, 1, 2], "B": [2, 3], "C": [3]}[which]
    pv = make_pvec(inp, layers)
    ws = stream_array(inp, plan)
    nc, P = build_launch(which, pv, plan, stop_after)
    print("launch", which, "instructions", P.ninst, "waits", P.nwait, "slots", ws.shape[0], flush=True)
    cst = make_consts()
    pva = pv.array()
    in_maps = []
    for r in range(NCORE):
        m = {"xin": state["x"][r], "pvec": pva, "cst": cst, "wstream": ws}
        if which == "A":
            m["vm"] = state["vm"][r]
        if which in ("A", "B") and stop_after is None:
            c_, s_ = rope_tables(r)
            m["ropec"], m["ropes"] = c_, s_
        if which in ("B", "C"):
            m["modst_i"] = state["modst"][r]
            m["qd_i"] = state["q"][r]
            m["k_all"] = state["k_all"]
            m["v_all"] = state["v_all"]
        in_maps.append(m)
    res = run_bass_kernel_spmd(nc, in_maps, core_ids=list(range(NCORE)), trace=trace)
    if trace:
        print("exec_time_ns", res.exec_time_ns, flush=True)
    out = {"x": [res.results[r]["xout"] for r in range(NCORE)]}
    if which in ("A", "B"):
        out["modst"] = [res.results[r]["modst_o"] for r in range(NCORE)]
    if which in ("A", "B") and stop_after is None:
        out["q"] = [res.results[r]["qd_o"] for r in range(NCORE)]
        out["k_all"] = np.concatenate([res.results[r]["kd_o"] for r in range(NCORE)], axis=0)
        out["v_all"] = np.concatenate([res.results[r]["vd_o"] for r in range(NCORE)], axis=0)
    return out


def kernel(**inp):
    inp = {k_: np.asarray(v) for k_, v in inp.items()}
    xs, vms = prep_A(inp)
    st = run_launch("A", inp, {"x": xs, "vm": vms})
    st = run_launch("B", inp, st)
    st = run_launch("C", inp, st)
    out = np.empty((1, SEQ, D), np.float32)
    for r in range(NCORE):
        o = st["x"][r]
        out[0, r * LAT_PC:(r + 1) * LAT_PC] = o[:, :, 0:LAT_PC].transpose(2, 1, 0).reshape(LAT_PC, D)
    return out
```

```python
import math
import numpy as np
import concourse.bass as bass
import concourse.mybir as mybir
from concourse.bass_utils import run_bass_kernel_spmd

F32 = mybir.dt.float32
F32R = mybir.dt.float32r
BF16 = mybir.dt.bfloat16
AF = mybir.ActivationFunctionType
ALU = mybir.AluOpType
AX = mybir.AxisListType

D = 2048
NCH = 16
SEQ = 8192
CTX = 256
NCORE = 8
LAT_PC = SEQ // NCORE
CTX_PC = CTX // NCORE
HALO = 9
NLA = LAT_PC + 2 * HALO
NCA = CTX_PC + 2 * HALO
TA = NLA + NCA
TB = LAT_PC + CTX_PC
DEPTH = 4
ALPHA = (2.0 * DEPTH) ** 0.25
LN_EPS = 1e-6
RMS_EPS = 1e-6
NE = 16
FF = 768
NF = FF // 128
GRID_W = 64
HD = 128
SLOT = 4096
RING = 5
NSCR = 3
SAME_ENG_SYNC = True


class Buf:
    __slots__ = ("w", "r", "dsem", "dcnt", "name")

    def __init__(self, name=""):
        self.w = None
        self.r = []
        self.dsem = None
        self.dcnt = 0
        self.name = name


class Eng:
    def __init__(self, P, name, e, has_sem=True):
        self.name = name
        self.e = e
        self.sem = P.nc.alloc_semaphore("sem_" + name) if has_sem else None
        self.cnt = 0
        self.seen = {}
        self.ispe = name == "pe"
        self.last_signaled = True


class Prog:
    def __init__(self, nc):
        self.nc = nc
        self.pe = Eng(self, "pe", nc.tensor)
        self.act = Eng(self, "act", nc.scalar)
        self.dve = Eng(self, "dve", nc.vector)
        self.pool = Eng(self, "pool", nc.gpsimd)
        self.sp = Eng(self, "sp", nc.sync, has_sem=False)
        self.sems = []
        self.nwait = 0
        self.ninst = 0

    def _deps(self, reads, writes):
        deps = {}

        def add(ev):
            if ev is None:
                return
            k = id(ev[0])
            if k not in deps or deps[k][1] < ev[1]:
                deps[k] = ev

        for b in reads:
            add(b.w)
        for b in writes:
            add(b.w)
            for ev in b.r:
                add(ev)
        return deps

    def _wait(self, E, deps):
        for k, (sem, val) in deps.items():
            if sem is E.sem and (E.ispe or not SAME_ENG_SYNC):
                continue
            if E.seen.get(k, 0) >= val:
                continue
            E.e.wait_ge(sem, val)
            E.seen[k] = val
            self.nwait += 1

    def op(self, E, fn, reads=(), writes=(), signal=True):
        self._wait(E, self._deps(reads, writes))
        ins = fn(E.e)
        self.ninst += 1
        if signal:
            E.cnt += 1
            ins.then_inc(E.sem, 1)
            ev = (E.sem, E.cnt)
            E.last_signaled = True
        else:
            ev = (E.sem, E.cnt + 1)
            E.last_signaled = False
        for b in reads:
            b.r.append(ev)
        for b in writes:
            b.w = ev
            b.r = []
        return ins

    def dma(self, Q, out, in_, reads=(), writes=(), dbuf=None, **kw):
        self._wait(Q, self._deps(reads, writes))
        ins = Q.e.dma_start(out=out, in_=in_, **kw)
        self.ninst += 1
        b = dbuf
        if b.dsem is None:
            b.dsem = self.nc.alloc_semaphore("dsem%d" % len(self.sems))
            self.sems.append(b.dsem)
        b.dcnt += 16
        ins.then_inc(b.dsem, 16)
        ev = (b.dsem, b.dcnt)
        for r in reads:
            r.r.append(ev)
        for w in writes:
            w.w = ev
            w.r = []
        return ev

    def fix(self, bufs, dbuf):
        ev = (dbuf.dsem, dbuf.dcnt)
        for b in bufs:
            b.w = ev

    def barrier(self, bufs=()):
        engs = [self.pe, self.act, self.dve, self.pool]
        assert self.pe.last_signaled
        deps = self._deps((), bufs)
        for E in engs + [self.sp]:
            d = dict(deps)
            for F in engs:
                if F is not E and F.cnt > 0:
                    d[id(F.sem)] = (F.sem, F.cnt)
            self._wait(E, d)

    def wait_all(self, E, bufs):
        deps = self._deps((), bufs)
        self._wait(E, deps)


class TT:
    def __init__(self, P, name, shape, dtype, nbuf=1, psum=False, handle=None):
        if handle is not None:
            self.t = handle
        elif psum:
            self.t = P.nc.alloc_psum_tensor(name, shape, dtype)
        else:
            self.t = P.nc.alloc_sbuf_tensor(name, shape, dtype)
        self.b = [Buf(name + str(i)) for i in range(nbuf)]

    def __getitem__(self, idx):
        return self.t[idx]


class Phase:
    _n = 0

    def __init__(self, P):
        self.P = P
        self.guards = []
        self.tts = []

    def tt(self, name, shape, dtype, nbuf=1):
        Phase._n += 1
        g = self.P.nc.sbuf_tensor("%s_%d" % (name, Phase._n), shape, dtype)
        h = g.__enter__()
        self.guards.append(g)
        t = TT(self.P, name, shape, dtype, nbuf=nbuf, handle=h)
        self.tts.append(t)
        return t

    def close(self):
        P = self.P
        bufs = [b for t in self.tts for b in t.b]
        P.barrier(bufs)
        for g in reversed(self.guards):
            g.__exit__(None, None, None)


def chunked(v):
    v = np.asarray(v, np.float32).reshape(-1)
    n = v.shape[0] // 128
    return np.ascontiguousarray(v.reshape(n, 128).T)


def proj_slot(W, col_starts):
    out = np.zeros((128, 16, 256), np.float32)
    for i, c0 in enumerate(col_starts):
        out[:, :, i * 128:(i + 1) * 128] = W[:, c0:c0 + 128].reshape(16, 128, 128).transpose(1, 0, 2)
    return out.reshape(128, SLOT)


def build_slot(inp, spec):
    kind = spec[0]
    if kind == "mod":
        _, i, j = spec
        return proj_slot(inp["mod_w"][i], [j * 256, j * 256 + 128])
    if kind == "proj":
        _, name, j, cols = spec
        return proj_slot(inp[name][j], cols)
    if kind == "pool":
        _, g0 = spec
        out = np.zeros((128, 2, 4, 512), np.float32)
        for gi in range(2):
            out[:, gi] = inp["pool_w"][0][g0 + gi].reshape(4, 128, 512).transpose(1, 0, 2)
        return out.reshape(128, SLOT)
    if kind == "router":
        _, i = spec
        W = np.concatenate([inp["rt_grp_w"][i], inp["rt_exp_w"][i]], axis=1)
        out = np.zeros((128, SLOT), np.float32)
        out[:, :320] = W.reshape(16, 128, 20).transpose(1, 0, 2).reshape(128, 320)
        return out
    if kind == "up":
        _, i, e, f = spec
        out = np.zeros((128, 16, 256), np.float32)
        out[:, :, 0:128] = inp["ex_w_gate"][i, e][:, f * 128:(f + 1) * 128].reshape(16, 128, 128).transpose(1, 0, 2)
        out[:, :, 128:256] = inp["ex_w_up"][i, e][:, f * 128:(f + 1) * 128].reshape(16, 128, 128).transpose(1, 0, 2)
        return out.reshape(128, SLOT)
    if kind == "down":
        _, i, e, f0 = spec
        out = np.zeros((128, 2, 2048), np.float32)
        for k in range(2):
            out[:, k] = inp["ex_w_down"][i, e][(f0 + k) * 128:(f0 + k + 1) * 128, :]
        return out.reshape(128, SLOT)
    if kind == "vproj":
        _, name, j, c0, col0 = spec
        W = inp[name][j]
        out = W[c0 * 128:(c0 + 8) * 128, col0:col0 + 512].reshape(8, 128, 512).transpose(1, 0, 2)
        return np.ascontiguousarray(out).reshape(128, SLOT)
    raise ValueError(kind)


class PVec:
    def __init__(self):
        self.cols = []
        self.off = {}
        self.n = 0

    def add(self, name, arr):
        arr = np.asarray(arr, np.float32)
        assert arr.shape[0] == 128
        arr = arr.reshape(128, -1)
        self.off[name] = (self.n, arr.shape[1])
        self.cols.append(arr)
        self.n += arr.shape[1]

    def array(self):
        return np.ascontiguousarray(np.concatenate(self.cols, axis=1))


def make_pvec(inp, layers):
    pv = PVec()
    pv.add("c", chunked(inp["c"][0]))
    pv.add("cctx", chunked(inp["c_ctx"]))
    for i in layers:
        pv.add("mod_b%d" % i, chunked(inp["mod_b"][i]))
        for k in range(2):
            pv.add("ln_g%d_%d" % (i, k), chunked(inp["ln_g"][i, k]))
            pv.add("ln_b%d_%d" % (i, k), chunked(inp["ln_b"][i, k]))
        rb = np.concatenate([inp["rt_grp_b"][i], inp["rt_exp_b"][i]])
        pv.add("rtb%d" % i, np.broadcast_to(rb[None, :], (128, 20)))
    if 0 in layers:
        for k in range(3):
            pv.add("conv_w%d" % k, chunked(inp["conv_w"][0, k]))
    if 1 in layers:
        pv.add("pool_scale", chunked(inp["pool_scale"][0]))
    if 2 in layers:
        pv.add("qk_norm", np.ascontiguousarray(inp["gqa_qk_norm"][0].T))
    if 3 in layers:
        pv.add("lam", np.ascontiguousarray(inp["diff_lambda"][0].T))
        pv.add("subln", chunked(inp["diff_subln"][0]))
    return pv


def make_consts():
    c = np.zeros((128, 128 * 3), np.float32)
    c[:, 0:128] = np.eye(128, dtype=np.float32)
    c[:, 128:256] = 1.0
    L = np.zeros((128, 128), np.float32)
    for i in range(32):
        L[32 + i, i] = -1.0
        L[i, 32 + i] = 1.0
        L[96 + i, 64 + i] = -1.0
        L[64 + i, 96 + i] = 1.0
    c[:, 256:384] = L
    return c


C_IDENT, C_ONES, C_ROT, C_SEL = 0, 128, 256, 384


def plan_mod(i):
    return [("mod", i, j) for j in range(48)]


def plan_moe(i, side=None):
    p = [("router", i)]
    for e in range(NE):
        for f in range(NF):
            p.append(("up", i, e, f))
            if side is not None and f % 2 == 1:
                p.append(("mod", side, e * 3 + f // 2))
        for f0 in range(0, NF, 2):
            p.append(("down", i, e, f0))
    return p


def plan_layer0():
    p = []
    for c in range(16):
        if c % 2 == 0:
            p.append(("proj", "conv_in_w", 0, [c * 128, (c + 1) * 128]))
        p.append(("proj", "conv_in_w", 0, [2048 + c * 128, 4096 + c * 128]))
    for d in range(0, 16, 2):
        p.append(("proj", "conv_out_w", 0, [d * 128, (d + 1) * 128]))
    return p


def plan_layer1():
    return [("pool", 0), ("pool", 2)]


class WStream:
    def __init__(self, P, plan, wdram):
        self.P = P
        self.plan = plan
        self.w = wdram
        self.issued = 0
        self.cur = 0
        self.released = [True] * RING
        self.didx = []
        n = 0
        for sp in plan:
            self.didx.append(n)
            if sp[0] != "fence":
                n += 1
        self._alloc()

    def _alloc(self):
        Phase._n += 1
        self.guard = self.P.nc.sbuf_tensor("wring_%d" % Phase._n, [128, RING, SLOT], BF16)
        h = self.guard.__enter__()
        self.ring = TT(self.P, "wring", [128, RING, SLOT], BF16, nbuf=RING, handle=h)
        self.released = [True] * RING

    def _pump(self):
        while self.issued < len(self.plan) and self.issued < self.cur + RING:
            j = self.issued
            if self.plan[j][0] == "fence":
                break
            s = j % RING
            if not self.released[s]:
                break
            b = self.ring.b[s]
            self.P.dma(self.P.pool, self.ring[:, s, :].rearrange("p (a b) -> p a b", b=2048),
                       self.w[self.didx[j]].rearrange("p (a b) -> p a b", b=2048), writes=[b], dbuf=b)
            self.released[s] = False
            self.issued += 1

    def get(self, kind):
        i = self.cur
        assert i < len(self.plan), "stream exhausted"
        assert self.plan[i][0] == kind, (self.plan[i], kind)
        self._pump()
        assert self.issued > i, "ring deadlock: slot not released"
        self.cur += 1
        s = i % RING
        return s, self.ring.b[s]

    def release(self, slot):
        self.released[slot[0]] = True
        self._pump()

    def suspend(self):
        assert self.plan[self.cur][0] == "fence" and self.issued == self.cur, (self.cur, self.issued)
        assert all(self.released)
        self.P.barrier(self.ring.b)
        self.guard.__exit__(None, None, None)

    def resume(self):
        self._alloc()
        self.cur += 1
        self.issued += 1


def stream_array(inp, plan):
    specs = [sp for sp in plan if sp[0] != "fence"]
    ws = np.empty((len(specs), 128, SLOT), np.float32)
    for n, spec in enumerate(specs):
        ws[n] = build_slot(inp, spec)
    return ws


class K:
    pass


def tok_tiles(T, n=3):
    w = (T + n - 1) // n
    return [(i * w, min(T, (i + 1) * w)) for i in range(n) if i * w < T]


def cls_split(lo, hi, nl):
    out = []
    if lo < nl:
        out.append((lo, min(hi, nl), 0))
    if hi > nl:
        out.append((max(lo, nl), hi, 1))
    return out


def setup_common(P, k, T, nl, pv, layers, pvd, cstd):
    nc = P.nc
    k.T, k.nl = T, nl
    k.tiles = tok_tiles(T)
    k.pv = pv
    k.PV = TT(P, "PV", [128, pv.n], F32)
    k.CST = TT(P, "CST", [128, 384], F32)
    ld = Buf("ld")
    P.dma(P.sp, k.PV[:, :], pvd, writes=[k.PV.b[0]], dbuf=ld)
    P.dma(P.sp, k.CST[:, :], cstd, writes=[k.CST.b[0]], dbuf=ld)
    P.fix([k.PV.b[0], k.CST.b[0]], ld)
    k.X = TT(P, "X", [128, NCH, T], F32, nbuf=NCH)
    k.H = TT(P, "H", [128, NCH, T], BF16, nbuf=NCH)
    k.PS = [TT(P, "ps%d" % i, [128, 512], F32, psum=True) for i in range(8)]
    k.psi = 0
    k.ps_res = set()
    k.MOD = TT(P, "MOD", [128, len(layers), 2, 96], F32)
    k.DER = TT(P, "DER", [128, len(layers), 2, 6, 16], F32)
    k.lidx = {l: n for n, l in enumerate(layers)}
    k.SCR = TT(P, "SCR", [128, NSCR, 512], F32, nbuf=NSCR)
    k.scri = 0
    k.zni = 0
    k.EPS = TT(P, "EPS", [128, 4], F32)
    k.IDB = TT(P, "IDB", [128, 128], BF16)
    k.ONEB = TT(P, "ONEB", [128, 128], BF16)
    k.SBT = TT(P, "SBT", [128, NCH, 2], BF16)
    P.op(P.dve, lambda e: e.memset(k.EPS[:, 0:1], LN_EPS / (ALPHA * ALPHA)), writes=[k.EPS.b[0]])
    P.op(P.dve, lambda e: e.memset(k.EPS[:, 1:2], RMS_EPS), writes=[k.EPS.b[0]])
    P.op(P.dve, lambda e: e.memset(k.EPS[:, 2:3], 0.0), writes=[k.EPS.b[0]])
    P.op(P.dve, lambda e: e.tensor_copy(out=k.IDB[:, :], in_=k.CST[:, C_IDENT:C_IDENT + 128]),
         reads=[k.CST.b[0]], writes=[k.IDB.b[0]])
    P.op(P.dve, lambda e: e.tensor_copy(out=k.ONEB[:, :], in_=k.CST[:, C_ONES:C_ONES + 128]),
         reads=[k.CST.b[0]], writes=[k.ONEB.b[0]])
    k.ONER = TT(P, "ONER", [128, 128], F32)
    P.op(P.dve, lambda e: e.tensor_copy(out=k.ONER[:, :].bitcast(F32R), in_=k.CST[:, C_ONES:C_ONES + 128]),
         reads=[k.CST.b[0]], writes=[k.ONER.b[0]])


def psum(k):
    while True:
        t = k.PS[k.psi % 8]
        k.psi += 1
        if id(t) not in k.ps_res:
            return t


def ps_reserve(k):
    t = psum(k)
    k.ps_res.add(id(t))
    return t


def ps_unreserve(k, t):
    k.ps_res.discard(id(t))


def scr(k):
    i = k.scri % NSCR
    k.scri += 1
    return k.SCR[:, i, :], k.SCR.b[i]


def pvs(k, name, lo=None, hi=None):
    o, w = k.pv.off[name]
    if lo is None:
        return k.PV[:, o:o + w]
    return k.PV[:, o + lo:o + hi]


def emit_mod(P, k, W, i):
    for _ in mod_steps(P, k, W, i):
        pass


def mod_steps(P, k, W, i):
    li = k.lidx[i]
    ps = ps_reserve(k)
    for j2 in range(48):
        s, sb = W.get("mod")
        for jj in range(2):
            j = j2 * 2 + jj
            for c in range(NCH):
                P.op(P.pe, lambda e, c=c, jj=jj, j=j, s=s: e.matmul(
                    ps[:, 2 * j:2 * j + 2], W.ring[:, s, c * 256 + jj * 128:c * 256 + jj * 128 + 128],
                    k.SBT[:, c, :],
                    start=(c == 0), stop=(c == NCH - 1)),
                    reads=[sb, k.SBT.b[0]], writes=[ps.b[0]], signal=(c == NCH - 1))
        W.release((s, sb))
        yield
    o, w = k.pv.off["mod_b%d" % i]
    for cls in range(2):
        P.op(P.dve, lambda e, cls=cls: e.tensor_tensor(
            out=k.MOD[:, li, cls, :], in0=ps[:, 0:192].rearrange("p (j c) -> p j c", c=2)[:, :, cls],
            in1=k.PV[:, o:o + 96], op=ALU.add),
            reads=[ps.b[0], k.PV.b[0]], writes=[k.MOD.b[0]])
    g1 = pvs(k, "ln_g%d_0" % i)
    b1 = pvs(k, "ln_b%d_0" % i)
    for cls in range(2):
        M = lambda m: k.MOD[:, li, cls, m * 16:(m + 1) * 16]
        Dv = lambda n: k.DER[:, li, cls, n, :]
        rw = dict(reads=[k.MOD.b[0], k.PV.b[0], k.DER.b[0]], writes=[k.DER.b[0]])
        P.op(P.dve, lambda e: e.tensor_scalar(out=Dv(0), in0=M(1), scalar1=1.0, scalar2=None, op0=ALU.add), **rw)
        P.op(P.dve, lambda e: e.tensor_copy(out=Dv(1), in_=M(0)), **rw)
        P.op(P.dve, lambda e: e.tensor_scalar(out=Dv(2), in0=M(2), scalar1=1.0 / ALPHA, scalar2=None, op0=ALU.mult), **rw)
        P.op(P.dve, lambda e: e.scalar_tensor_tensor(out=Dv(3), in0=M(4), scalar=1.0, in1=g1, op0=ALU.add, op1=ALU.mult), **rw)
        P.op(P.dve, lambda e: e.scalar_tensor_tensor(out=Dv(4), in0=M(4), scalar=1.0, in1=b1, op0=ALU.add, op1=ALU.mult), **rw)
        P.op(P.dve, lambda e: e.tensor_tensor(out=Dv(4), in0=Dv(4), in1=M(3), op=ALU.add), **rw)
        P.op(P.dve, lambda e: e.tensor_scalar(out=Dv(5), in0=M(5), scalar1=1.0 / ALPHA, scalar2=None, op0=ALU.mult), **rw)
    ps_unreserve(k, ps)
    yield


def emit_silu_c(P, k):
    for cls, name in enumerate(["c", "cctx"]):
        P.op(P.act, lambda e, cls=cls, name=name: e.activation(out=k.SBT[:, :, cls], in_=pvs(k, name), func=AF.Silu),
             reads=[k.PV.b[0]], writes=[k.SBT.b[0]])


def emit_modulate(P, k, i):
    li = k.lidx[i]
    for c in range(NCH):
        for (lo, hi, cls) in cls_split(0, k.T, k.nl):
            eng = P.dve if (c % 2 == 0) else P.pool
            P.op(eng, lambda e, c=c, lo=lo, hi=hi, cls=cls: e.tensor_scalar(
                out=k.H[:, c, lo:hi], in0=k.X[:, c, lo:hi],
                scalar1=k.DER[:, li, cls, 0, c:c + 1], scalar2=k.DER[:, li, cls, 1, c:c + 1],
                op0=ALU.mult, op1=ALU.add),
                reads=[k.X.b[c], k.DER.b[0]], writes=[k.H.b[c]])


def emit_ln(P, k, i, which, make_h):
    li = k.lidx[i]
    onesr = k.ONER[:, :].bitcast(F32R)
    T = k.T
    ph = Phase(P)
    k.MEAN = ph.tt("MEAN", [128, T], F32)
    k.RSTD = ph.tt("RSTD", [128, T], F32)
    k.ZN = ph.tt("ZN", [128, 2, T], F32, nbuf=2)
    SQ = ph.tt("LSQ", [128, 2, 512], F32, nbuf=2)
    XR = ph.tt("LXR", [128, 2, 512], F32, nbuf=2)
    for (lo, hi) in k.tiles:
        n = hi - lo
        ps1, ps2 = psum(k), psum(k)
        for c in range(NCH):
            z = c % 2
            P.op(P.act, lambda e, c=c, z=z: e.activation(out=SQ[:, z, :n].bitcast(F32R), in_=k.X[:, c, lo:hi], func=AF.Square),
                 reads=[k.X.b[c]], writes=[SQ.b[z]])
            eng = P.dve if c % 2 == 0 else P.pool
            P.op(eng, lambda e, c=c, z=z: e.tensor_copy(out=XR[:, z, :n].bitcast(F32R), in_=k.X[:, c, lo:hi]),
                 reads=[k.X.b[c]], writes=[XR.b[z]])
            P.op(P.pe, lambda e, c=c, z=z: e.matmul(ps1[:, :n], onesr, XR[:, z, :n].bitcast(F32R), start=(c == 0), stop=(c == NCH - 1)),
                 reads=[XR.b[z], k.ONER.b[0]], writes=[ps1.b[0]], signal=True)
            P.op(P.pe, lambda e, c=c, z=z: e.matmul(ps2[:, :n], onesr, SQ[:, z, :n].bitcast(F32R), start=(c == 0), stop=(c == NCH - 1)),
                 reads=[SQ.b[z], k.ONER.b[0]], writes=[ps2.b[0]], signal=True)
        P.op(P.act, lambda e: e.mul(out=k.MEAN[:, lo:hi], in_=ps1[:, :n], mul=1.0 / D),
             reads=[ps1.b[0]], writes=[k.MEAN.b[0]])
        msq, msqb = scr(k)
        P.op(P.dve, lambda e: e.tensor_tensor(out=msq[:, :n], in0=k.MEAN[:, lo:hi], in1=k.MEAN[:, lo:hi], op=ALU.mult),
             reads=[k.MEAN.b[0]], writes=[msqb])
        P.op(P.dve, lambda e: e.scalar_tensor_tensor(out=msq[:, :n], in0=ps2[:, :n], scalar=1.0 / D, in1=msq[:, :n],
                                                     op0=ALU.mult, op1=ALU.subtract),
             reads=[ps2.b[0], msqb], writes=[msqb])
        P.op(P.act, lambda e: e.activation(out=msq[:, :n], in_=msq[:, :n], func=AF.Sqrt, bias=k.EPS[:, 0:1]),
             reads=[msqb, k.EPS.b[0]], writes=[msqb])
        P.op(P.dve, lambda e: e.reciprocal(out=k.RSTD[:, lo:hi], in_=msq[:, :n]),
             reads=[msqb], writes=[k.RSTD.b[0]])
    g = pvs(k, "ln_g%d_%d" % (i, which))
    b = pvs(k, "ln_b%d_%d" % (i, which))
    for c in range(NCH):
        z = k.zni % 2
        k.zni += 1
        e1 = P.dve if c % 4 == 3 else P.pool
        P.op(e1, lambda e, c=c, z=z: e.tensor_tensor(out=k.ZN[:, z, :], in0=k.X[:, c, 0:T], in1=k.MEAN[:, :], op=ALU.subtract),
             reads=[k.X.b[c], k.MEAN.b[0]], writes=[k.ZN.b[z]])
        P.op(P.dve, lambda e, z=z: e.tensor_tensor(out=k.ZN[:, z, :], in0=k.ZN[:, z, :], in1=k.RSTD[:, :], op=ALU.mult),
             reads=[k.ZN.b[z], k.RSTD.b[0]], writes=[k.ZN.b[z]])
        P.op(P.act, lambda e, c=c, z=z: e.activation(out=k.X[:, c, 0:T], in_=k.ZN[:, z, :], func=AF.Identity,
                                                     scale=g[:, c:c + 1], bias=b[:, c:c + 1]),
             reads=[k.ZN.b[z], k.PV.b[0]], writes=[k.X.b[c]])
        if make_h:
            for (lo, hi, cls) in cls_split(0, T, k.nl):
                P.op(P.act, lambda e, c=c, z=z, lo=lo, hi=hi, cls=cls: e.activation(
                    out=k.H[:, c, lo:hi], in_=k.ZN[:, z, lo:hi], func=AF.Identity,
                    scale=k.DER[:, li, cls, 3, c:c + 1], bias=k.DER[:, li, cls, 4, c:c + 1]),
                    reads=[k.ZN.b[z], k.DER.b[0]], writes=[k.H.b[c]])
    ph.close()


def emit_resid(P, k, ps, d, lo, hi, scal_fn, extra_reads=()):
    for (a, b_, cls) in cls_split(lo, hi, k.nl):
        P.op(P.dve, lambda e, a=a, b_=b_, cls=cls: e.scalar_tensor_tensor(
            out=k.X[:, d, a:b_], in0=ps[:, a - lo:b_ - lo], scalar=scal_fn(cls, d), in1=k.X[:, d, a:b_],
            op0=ALU.mult, op1=ALU.add),
            reads=[ps.b[0], k.X.b[d], k.DER.b[0]] + list(extra_reads), writes=[k.X.b[d]])


def emit_moe(P, k, W, i, side=None):
    li = k.lidx[i]
    T = k.T
    nt128 = (T + 127) // 128
    ph = Phase(P)
    k.COMBT = ph.tt("COMBT", [16, T], F32)
    k.CM = ph.tt("CM", [16, T], F32)
    k.CB = ph.tt("CB", [128, T], F32)
    k.ACTB = ph.tt("ACTB", [128, NF, T], BF16, nbuf=NF)
    s, sb = W.get("router")
    rtb = pvs(k, "rtb%d" % i)
    ident = k.CST[:, C_IDENT:C_IDENT + 128]
    NRG = 5
    RT = ph.tt("RT", [128, NRG, 96], F32, nbuf=NRG)

    def route_tile(t):
        lo, hi = t * 128, min(T, (t + 1) * 128)
        n = hi - lo
        ps = psum(k)
        for c in range(NCH):
            P.op(P.pe, lambda e, c=c: e.matmul(ps[:n, 0:20], k.H[:, c, lo:hi], W.ring[:, s, c * 20:(c + 1) * 20],
                                               start=(c == 0), stop=(c == NCH - 1)),
                 reads=[k.H.b[c], sb], writes=[ps.b[0]], signal=(c == NCH - 1))
        R, Rb = RT[:, t % NRG, :], RT.b[t % NRG]
        yield
        rw = dict(reads=[Rb], writes=[Rb])
        yield
        LG = R[:n, 0:20]
        P.op(P.dve, lambda e: e.tensor_tensor(out=LG, in0=ps[:n, 0:20], in1=rtb[:n, :], op=ALU.add),
             reads=[ps.b[0], k.PV.b[0]], writes=[Rb])
        yield
        gmax = R[:n, 20:21]
        P.op(P.dve, lambda e: e.tensor_reduce(out=gmax, in_=R[:n, 0:4], axis=AX.X, op=ALU.max), **rw)
        yield
        oh = R[:n, 24:28]
        P.op(P.dve, lambda e: e.tensor_scalar(out=oh, in0=R[:n, 0:4], scalar1=gmax, scalar2=None, op0=ALU.is_ge), **rw)
        yield
        ngm = R[:n, 21:22]
        P.op(P.dve, lambda e: e.tensor_scalar(out=ngm, in0=gmax, scalar1=-1.0, scalar2=None, op0=ALU.mult), **rw)
        yield
        ex = R[:n, 28:32]
        ssum = R[:n, 22:23]
        P.op(P.act, lambda e: e.activation(out=ex, in_=R[:n, 0:4], func=AF.Exp, bias=ngm, accum_out=ssum), **rw)
        yield
        pg = R[:n, 23:24]
        P.op(P.dve, lambda e: e.reciprocal(out=pg, in_=ssum), **rw)
        yield
        les = R[:n, 48:52]
        P.op(P.dve, lambda e: e.tensor_scalar(out=les, in0=R[:n, 4:8], scalar1=R[:n, 24:25], scalar2=None, op0=ALU.mult), **rw)
        yield
        for g in range(1, 4):
            P.op(P.dve, lambda e, g=g: e.scalar_tensor_tensor(out=les, in0=R[:n, 4 + 4 * g:8 + 4 * g], scalar=R[:n, 24 + g:25 + g],
                                                              in1=les, op0=ALU.mult, op1=ALU.add), **rw)
        m1 = R[:n, 52:53]
        P.op(P.dve, lambda e: e.tensor_reduce(out=m1, in_=les, axis=AX.X, op=ALU.max), **rw)
        yield
        k1 = R[:n, 56:60]
        P.op(P.dve, lambda e: e.tensor_scalar(out=k1, in0=les, scalar1=m1, scalar2=None, op0=ALU.is_ge), **rw)
        yield
        le2 = R[:n, 60:64]
        P.op(P.dve, lambda e: e.scalar_tensor_tensor(out=le2, in0=k1, scalar=-1e30, in1=les, op0=ALU.mult, op1=ALU.add), **rw)
        yield
        m2 = R[:n, 53:54]
        P.op(P.dve, lambda e: e.tensor_reduce(out=m2, in_=le2, axis=AX.X, op=ALU.max), **rw)
        yield
        k2 = R[:n, 64:68]
        P.op(P.dve, lambda e: e.tensor_scalar(out=k2, in0=le2, scalar1=m2, scalar2=None, op0=ALU.is_ge), **rw)
        yield
        dm = R[:n, 54:55]
        P.op(P.dve, lambda e: e.tensor_tensor(out=dm, in0=m2, in1=m1, op=ALU.subtract), **rw)
        yield
        w2 = R[:n, 55:56]
        P.op(P.act, lambda e: e.activation(out=w2, in_=dm, func=AF.Sigmoid), **rw)
        yield
        w1 = R[:n, 68:69]
        P.op(P.dve, lambda e: e.tensor_scalar(out=w1, in0=w2, scalar1=-1.0, scalar2=1.0, op0=ALU.mult, op1=ALU.add), **rw)
        yield
        cs = R[:n, 72:76]
        P.op(P.dve, lambda e: e.tensor_scalar(out=cs, in0=k1, scalar1=w1, scalar2=None, op0=ALU.mult), **rw)
        yield
        P.op(P.dve, lambda e: e.scalar_tensor_tensor(out=cs, in0=k2, scalar=w2, in1=cs, op0=ALU.mult, op1=ALU.add), **rw)
        yield
        P.op(P.dve, lambda e: e.tensor_scalar(out=cs, in0=cs, scalar1=pg, scalar2=None, op0=ALU.mult), **rw)
        yield
        comb = R[:n, 80:96]
        for g in range(4):
            P.op(P.dve, lambda e, g=g: e.tensor_scalar(out=R[:n, 80 + 4 * g:84 + 4 * g], in0=cs, scalar1=R[:n, 24 + g:25 + g],
                                                       scalar2=None, op0=ALU.mult), **rw)
        pst = psum(k)
        P.op(P.pe, lambda e: e.transpose(pst[:16, :n], comb, ident[:n, :n]),
             reads=[Rb, k.CST.b[0]], writes=[pst.b[0]])
        yield
        P.op(P.act, lambda e: e.copy(out=k.COMBT[:16, lo:hi], in_=pst[:16, :n]),
             reads=[pst.b[0]], writes=[k.COMBT.b[0]])
        yield

    for g0 in range(0, nt128, NRG):
        gens = [route_tile(t) for t in range(g0, min(nt128, g0 + NRG))]
        while gens:
            for g_ in list(gens):
                try:
                    next(g_)
                except StopIteration:
                    gens.remove(g_)
    W.release((s, sb))
    for ex_ in range(NE):
        P.op(P.dve, lambda e: e.tensor_scalar(out=k.CM[:16, :], in0=k.COMBT[:16, :],
                                              scalar1=k.CST[0:16, C_IDENT + ex_:C_IDENT + ex_ + 1], scalar2=None, op0=ALU.mult),
             reads=[k.COMBT.b[0], k.CST.b[0]], writes=[k.CM.b[0]])
        for (lo, hi) in k.tiles:
            n = hi - lo
            ps = psum(k)
            P.op(P.pe, lambda e: e.matmul(ps[:, :n], k.CST[0:16, C_ONES:C_ONES + 128], k.CM[:16, lo:hi],
                                          start=True, stop=True),
                 reads=[k.CST.b[0], k.CM.b[0]], writes=[ps.b[0]])
            P.op(P.act, lambda e: e.copy(out=k.CB[:, lo:hi], in_=ps[:, :n]), reads=[ps.b[0]], writes=[k.CB.b[0]])
        for f in range(NF):
            s, sb = W.get("up")
            for (lo, hi) in k.tiles:
                n = hi - lo
                psg, psu = psum(k), psum(k)
                for which, ps in ((0, psg), (1, psu)):
                    for c in range(NCH):
                        P.op(P.pe, lambda e, c=c, which=which, ps=ps: e.matmul(
                            ps[:, :n], W.ring[:, s, c * 256 + which * 128:c * 256 + which * 128 + 128], k.H[:, c, lo:hi],
                            start=(c == 0), stop=(c == NCH - 1)),
                            reads=[sb, k.H.b[c]], writes=[ps.b[0]], signal=(c == NCH - 1))
                sg, sgb = scr(k)
                P.op(P.act, lambda e: e.activation(out=sg[:, :n], in_=psg[:, :n], func=AF.Silu),
                     reads=[psg.b[0]], writes=[sgb])
                P.op(P.pool, lambda e: e.tensor_tensor(out=sg[:, :n], in0=sg[:, :n], in1=k.CB[:, lo:hi], op=ALU.mult),
                     reads=[sgb, k.CB.b[0]], writes=[sgb])
                P.op(P.dve, lambda e: e.tensor_tensor(out=k.ACTB[:, f, lo:hi], in0=psu[:, :n], in1=sg[:, :n], op=ALU.mult),
                     reads=[psu.b[0], sgb], writes=[k.ACTB.b[f]])
            W.release((s, sb))
            if side is not None and f % 2 == 1:
                next(side, None)
        slots = [W.get("down") for _ in range(NF // 2)]
        for d in range(NCH):
            for (lo, hi) in k.tiles:
                n = hi - lo
                ps = psum(k)
                for f in range(NF):
                    s, sb = slots[f // 2]
                    off = (f % 2) * 2048 + d * 128
                    P.op(P.pe, lambda e, f=f, s=s, off=off: e.matmul(
                        ps[:, :n], W.ring[:, s, off:off + 128], k.ACTB[:, f, lo:hi], start=(f == 0), stop=(f == NF - 1)),
                        reads=[sb, k.ACTB.b[f]], writes=[ps.b[0]], signal=(f == NF - 1))
                emit_resid(P, k, ps, d, lo, hi, lambda cls, d: k.DER[:, li, cls, 5, d:d + 1])
        for sl in slots:
            W.release(sl)
    if side is not None:
        for _ in side:
            pass
    ph.close()


def emit_conv_mixer(P, k, W, i):
    li = k.lidx[i]
    T = k.T
    ph = Phase(P)
    G = ph.tt("G", [128, NCH, T], BF16, nbuf=NCH)
    k.UM = ph.tt("UM", [128, T], F32)
    k.CV = ph.tt("CV", [128, T], F32)
    P.op(P.pool, lambda e: e.memset(k.CV[:, :], 0.0), writes=[k.CV.b[0]])
    gb_slot = None
    for c in range(NCH):
        if c % 2 == 0:
            gb_slot = W.get("proj")
        s, sb = W.get("proj")
        UM = k.UM
        for (lo, hi) in k.tiles:
            n = hi - lo
            psc, psv = psum(k), psum(k)
            for which, ps in ((0, psc), (1, psv)):
                for cc in range(NCH):
                    P.op(P.pe, lambda e, cc=cc, which=which, ps=ps: e.matmul(
                        ps[:, :n], W.ring[:, s, cc * 256 + which * 128:cc * 256 + which * 128 + 128], k.H[:, cc, lo:hi],
                        start=(cc == 0), stop=(cc == NCH - 1)),
                        reads=[sb, k.H.b[cc]], writes=[ps.b[0]], signal=(cc == NCH - 1))
            vm, vmb = scr(k)
            P.op(P.dve, lambda e: e.tensor_tensor(out=vm[:, :n], in0=psv[:, :n], in1=k.VM[:, lo:hi], op=ALU.mult),
                 reads=[psv.b[0], k.VM.b[0]], writes=[vmb])
            P.op(P.dve, lambda e: e.tensor_tensor(out=UM[:, lo:hi], in0=psc[:, :n], in1=vm[:, :n], op=ALU.mult),
                 reads=[psc.b[0], vmb], writes=[UM.b[0]])
        W.release((s, sb))
        CV = k.CV
        w0 = pvs(k, "conv_w0")[:, c:c + 1]
        w1 = pvs(k, "conv_w1")[:, c:c + 1]
        w2 = pvs(k, "conv_w2")[:, c:c + 1]
        rw = dict(reads=[UM.b[0], k.PV.b[0], CV.b[0]], writes=[CV.b[0]])
        P.op(P.pool, lambda e: e.tensor_scalar(out=CV[:, 1:T - 1], in0=UM[:, 0:T - 2], scalar1=w0, scalar2=None, op0=ALU.mult), **rw)
        P.op(P.dve, lambda e: e.scalar_tensor_tensor(out=CV[:, 1:T - 1], in0=UM[:, 1:T - 1], scalar=w1, in1=CV[:, 1:T - 1],
                                                     op0=ALU.mult, op1=ALU.add), **rw)
        P.op(P.dve, lambda e: e.scalar_tensor_tensor(out=CV[:, 1:T - 1], in0=UM[:, 2:T], scalar=w2, in1=CV[:, 1:T - 1],
                                                     op0=ALU.mult, op1=ALU.add), **rw)
        s2, sb2 = gb_slot
        which = c % 2
        for (lo, hi) in k.tiles:
            n = hi - lo
            ps = psum(k)
            for cc in range(NCH):
                P.op(P.pe, lambda e, cc=cc: e.matmul(
                    ps[:, :n], W.ring[:, s2, cc * 256 + which * 128:cc * 256 + which * 128 + 128], k.H[:, cc, lo:hi],
                    start=(cc == 0), stop=(cc == NCH - 1)),
                    reads=[sb2, k.H.b[cc]], writes=[ps.b[0]], signal=(cc == NCH - 1))
            P.op(P.dve, lambda e: e.tensor_tensor(out=G[:, c, lo:hi], in0=ps[:, :n], in1=CV[:, lo:hi], op=ALU.mult),
                 reads=[ps.b[0], CV.b[0]], writes=[G.b[c]])
        if c % 2 == 1:
            W.release(gb_slot)
    for d2 in range(0, NCH, 2):
        s, sb = W.get("proj")
        for which in range(2):
            d = d2 + which
            for (lo, hi) in k.tiles:
                n = hi - lo
                ps = psum(k)
                for cc in range(NCH):
                    P.op(P.pe, lambda e, cc=cc: e.matmul(
                        ps[:, :n], W.ring[:, s, cc * 256 + which * 128:cc * 256 + which * 128 + 128], G[:, cc, lo:hi],
                        start=(cc == 0), stop=(cc == NCH - 1)),
                        reads=[sb, G.b[cc]], writes=[ps.b[0]], signal=(cc == NCH - 1))
                emit_resid(P, k, ps, d, lo, hi, lambda cls, d: k.DER[:, li, cls, 2, d:d + 1])
        W.release((s, sb))
    ph.close()


POOL_WINDOWS = (2, 4, 8, 16)


def emit_pool_mixer(P, k, W, i):
    li = k.lidx[i]
    T = k.T
    ph = Phase(P)
    RC = ph.tt("RC", [128, T], F32)
    A = ph.tt("PA", [128, T], F32)
    Bt = ph.tt("PB", [128, T], F32)
    HM = ph.tt("HM", [128, T], F32)
    WS = ph.tt("WS", [128, T], F32)
    PSC = ph.tt("PSC", [128, 2, 16], F32)
    VM = k.VM
    for t_ in (RC, A, Bt, WS):
        P.op(P.pool, lambda e, t_=t_: e.memset(t_[:, :], 0.0), writes=[t_.b[0]])

    def wsum(eng, dst, src, widx):
        cur = src
        for st in range(widx + 1):
            o = dst if st == widx else (A if st % 2 == 0 else Bt)
            lo, hi = 1 << st, T - (1 << st) + 1
            if st == 0:
                in0, in1 = cur[:, 0:T - 1], cur[:, 1:T]
            else:
                sh = 1 << (st - 1)
                in0, in1 = cur[:, lo - sh:hi - sh], cur[:, lo + sh:hi + sh]
            P.op(eng, lambda e: e.tensor_tensor(out=o[:, lo:hi], in0=in0, in1=in1, op=ALU.add),
                 reads=[cur.b[0]], writes=[o.b[0]])
            cur = o

    for c in range(NCH):
        widx = c // 4
        if c % 4 == 0:
            wsum(P.pool, RC, VM, widx)
            P.op(P.dve, lambda e: e.tensor_scalar(out=RC[:, :], in0=RC[:, :], scalar1=1.0, scalar2=None, op0=ALU.max),
                 reads=[RC.b[0]], writes=[RC.b[0]])
            P.op(P.dve, lambda e: e.reciprocal(out=RC[:, :], in_=RC[:, :]), reads=[RC.b[0]], writes=[RC.b[0]])
        P.op(P.pool, lambda e: e.tensor_tensor(out=HM[:, :], in0=k.H[:, c, :], in1=VM[:, :], op=ALU.mult),
             reads=[k.H.b[c], VM.b[0]], writes=[HM.b[0]])
        wsum(P.pool if c % 2 else P.dve, WS, HM, widx)
        P.op(P.dve, lambda e: e.tensor_tensor(out=WS[:, :], in0=WS[:, :], in1=RC[:, :], op=ALU.mult),
             reads=[WS.b[0], RC.b[0]], writes=[WS.b[0]])
        P.op(P.dve, lambda e: e.tensor_tensor(out=k.H[:, c, :], in0=WS[:, :], in1=k.H[:, c, :], op=ALU.subtract),
             reads=[WS.b[0], k.H.b[c]], writes=[k.H.b[c]])
    for cls in range(2):
        P.op(P.dve, lambda e: e.tensor_tensor(out=PSC[:, cls, :], in0=k.DER[:, li, cls, 2, :], in1=pvs(k, "pool_scale"), op=ALU.mult),
             reads=[k.DER.b[0], k.PV.b[0]], writes=[PSC.b[0]])
    for g0 in (0, 2):
        s, sb = W.get("pool")
        for gi in range(2):
            g = g0 + gi
            for jo in range(4):
                d = g * 4 + jo
                for (lo, hi) in k.tiles:
                    n = hi - lo
                    ps = psum(k)
                    for kk in range(4):
                        off = gi * 2048 + kk * 512 + jo * 128
                        P.op(P.pe, lambda e, kk=kk, off=off: e.matmul(
                            ps[:, :n], W.ring[:, s, off:off + 128], k.H[:, g * 4 + kk, lo:hi], start=(kk == 0), stop=(kk == 3)),
                            reads=[sb, k.H.b[g * 4 + kk]], writes=[ps.b[0]], signal=(kk == 3))
                    emit_resid(P, k, ps, d, lo, hi, lambda cls, d: PSC[:, cls, d:d + 1], extra_reads=[PSC.b[0]])
        W.release((s, sb))
    ph.close()


SCALE = HD ** -0.5
LAM_INIT3 = 0.8 - 0.6 * math.exp(-0.3 * 3)
NKT = 66
KL = 8192


def emit_outproj(P, k, W, SRC, scal_fn, extra_reads=()):
    for d2 in range(0, NCH, 2):
        s, sb = W.get("proj")
        for which in range(2):
            d = d2 + which
            for (lo, hi) in k.tiles:
                n = hi - lo
                ps = psum(k)
                for cc in range(NCH):
                    P.op(P.pe, lambda e, cc=cc: e.matmul(
                        ps[:, :n], W.ring[:, s, cc * 256 + which * 128:cc * 256 + which * 128 + 128], SRC[:, cc, lo:hi],
                        start=(cc == 0), stop=(cc == NCH - 1)),
                        reads=[sb, SRC.b[cc]], writes=[ps.b[0]], signal=(cc == NCH - 1))
                emit_resid(P, k, ps, d, lo, hi, scal_fn, extra_reads=extra_reads)
        W.release((s, sb))


def emit_qkv(P, k, W, layer, qd, kd, vd, ropec, ropes):
    T, nl = k.T, k.nl
    gqa = layer == 2
    nq = 16
    nk = 4 if gqa else 16
    ph = Phase(P)
    RC_ = ph.tt("ROPEC", [128, nl], F32)
    RS_ = ph.tt("ROPES", [128, nl], F32)
    ldr = Buf("ldrope")
    P.dma(P.sp, RC_[:, :], ropec, writes=[RC_.b[0]], dbuf=ldr)
    P.dma(P.sp, RS_[:, :], ropes, writes=[RS_.b[0]], dbuf=ldr)
    P.fix([RC_.b[0], RS_.b[0]], ldr)
    QN = ph.tt("QN", [128, 2, 512], F32, nbuf=2)
    T1 = ph.tt("T1", [128, 2, 512], F32, nbuf=2)
    T2 = ph.tt("T2", [128, 2, 512], F32, nbuf=2)
    QK = ph.tt("QK", [128, 2, T], BF16, nbuf=2)
    VO = ph.tt("VO", [128, 2, 512], BF16, nbuf=2)
    GS = ph.tt("GS", [128, 2], F32)
    ones = k.CST[:, C_ONES:C_ONES + 128]
    rot = k.CST[:, C_ROT:C_ROT + 128]
    if gqa:
        P.op(P.dve, lambda e: e.tensor_scalar(out=GS[:, 0:1], in0=pvs(k, "qk_norm")[:, 0:1], scalar1=SCALE, scalar2=None, op0=ALU.mult),
             reads=[k.PV.b[0]], writes=[GS.b[0]])
        P.op(P.dve, lambda e: e.tensor_copy(out=GS[:, 1:2], in_=pvs(k, "qk_norm")[:, 1:2]),
             reads=[k.PV.b[0]], writes=[GS.b[0]])
    stq = [Buf("st_qk0"), Buf("st_qk1")]
    stv = [Buf("st_v0"), Buf("st_v1")]
    nmaps = nq + nk
    NT = len(k.tiles)
    items = [dict(m=m, ti=ti, lo=lo, hi=hi, idx=m * NT + ti) for m in range(nmaps) for ti, (lo, hi) in enumerate(k.tiles)]
    slots = {}

    def stageA(it):
        m, lo, hi = it["m"], it["lo"], it["hi"]
        n = hi - lo
        if m % 2 == 0 and it["ti"] == 0:
            slots[m // 2] = W.get("proj")
        s, sb = slots[m // 2]
        which = m % 2
        ps = psum(k)
        for cc in range(NCH):
            P.op(P.pe, lambda e, cc=cc: e.matmul(
                ps[:, :n], W.ring[:, s, cc * 256 + which * 128:cc * 256 + which * 128 + 128], k.H[:, cc, lo:hi],
                start=(cc == 0), stop=(cc == NCH - 1)),
                reads=[sb, k.H.b[cc]], writes=[ps.b[0]], signal=(cc == NCH - 1))
        it["ps"] = ps
        if gqa:
            sq, sqb = scr(k)
            P.op(P.act, lambda e: e.activation(out=sq[:, :n], in_=ps[:, :n], func=AF.Square), reads=[ps.b[0]], writes=[sqb])
            it["sq"], it["sqb"] = sq, sqb
        if m % 2 == 1 and it["ti"] == NT - 1:
            W.release(slots[m // 2])

    def stageB(it):
        m, lo, hi = it["m"], it["lo"], it["hi"]
        n = hi - lo
        isq = m < nq
        ps = it["ps"]
        qi = it["idx"] % 2
        qn, qnb = QN[:, qi, :], QN.b[qi]
        if gqa:
            sq, sqb = it["sq"], it["sqb"]
            pss = psum(k)
            P.op(P.pe, lambda e: e.matmul(pss[:, :n], ones, sq[:, :n], start=True, stop=True),
                 reads=[sqb, k.CST.b[0]], writes=[pss.b[0]])
            P.op(P.act, lambda e: e.activation(out=sq[:, :n], in_=pss[:, :n], func=AF.Sqrt, bias=k.EPS[:, 1:2], scale=1.0 / HD),
                 reads=[pss.b[0], k.EPS.b[0]], writes=[sqb])
            P.op(P.dve, lambda e: e.reciprocal(out=sq[:, :n], in_=sq[:, :n]), reads=[sqb], writes=[sqb])
            gcol = GS[:, 0:1] if isq else GS[:, 1:2]
            P.op(P.dve, lambda e: e.scalar_tensor_tensor(out=qn[:, :n], in0=ps[:, :n], scalar=gcol, in1=sq[:, :n],
                                                         op0=ALU.mult, op1=ALU.mult),
                 reads=[ps.b[0], sqb, GS.b[0]], writes=[qnb])
        else:
            P.op(P.act, lambda e: e.mul(out=qn[:, :n], in_=ps[:, :n], mul=(SCALE if isq else 1.0)),
                 reads=[ps.b[0]], writes=[qnb])

    def stageC(it):
        m, lo, hi = it["m"], it["lo"], it["hi"]
        isq = m < nq
        qi = it["idx"] % 2
        z = m % 2
        qn, qnb = QN[:, qi, :], QN.b[qi]
        for (a, b_, cls) in cls_split(lo, hi, nl):
            w_ = b_ - a
            o0 = a - lo
            if cls == 0:
                pr = psum(k)
                P.op(P.pe, lambda e: e.matmul(pr[:, :w_], rot, qn[:, o0:o0 + w_], start=True, stop=True),
                     reads=[qnb, k.CST.b[0]], writes=[pr.b[0]])
                P.op(P.pool, lambda e: e.tensor_tensor(out=T1[:, qi, :w_], in0=qn[:, o0:o0 + w_], in1=RC_[:, a:b_], op=ALU.mult),
                     reads=[qnb, RC_.b[0]], writes=[T1.b[qi]])
                P.op(P.dve, lambda e: e.tensor_tensor(out=T2[:, qi, :w_], in0=pr[:, :w_], in1=RS_[:, a:b_], op=ALU.mult),
                     reads=[pr.b[0], RS_.b[0]], writes=[T2.b[qi]])
                P.op(P.pool, lambda e: e.tensor_tensor(out=QK[:, z, a:b_], in0=T1[:, qi, :w_], in1=T2[:, qi, :w_], op=ALU.add),
                     reads=[T1.b[qi], T2.b[qi]], writes=[QK.b[z]])
            else:
                P.op(P.act, lambda e: e.copy(out=QK[:, z, a:b_], in_=qn[:, o0:o0 + w_]),
                     reads=[qnb], writes=[QK.b[z]])
        if it["ti"] == NT - 1:
            dst = qd[m * 128:(m + 1) * 128, :] if isq else kd[(m - nq) * 128:(m - nq + 1) * 128, :]
            P.dma(P.sp, dst, QK[:, z, 0:T], reads=[QK.b[z]], dbuf=stq[z])

    NI = len(items)
    for idx in range(NI + 2):
        if idx < NI:
            stageA(items[idx])
        if 0 <= idx - 1 < NI:
            stageB(items[idx - 1])
        if 0 <= idx - 2 < NI:
            stageC(items[idx - 2])
    ngrp = 1 if gqa else 4
    nt128 = (T + 127) // 128
    vcnt = 0
    for gi in range(ngrp):
        sl = [W.get("vproj"), W.get("vproj")]
        for t in range(nt128):
            lo, hi = t * 128, min(T, (t + 1) * 128)
            n = hi - lo
            ps = psum(k)
            for cc in range(NCH):
                s, sb = sl[cc // 8]
                P.op(P.pe, lambda e, cc=cc, s=s: e.matmul(
                    ps[:n, :], k.H[:, cc, lo:hi], W.ring[:, s, (cc % 8) * 512:(cc % 8 + 1) * 512],
                    start=(cc == 0), stop=(cc == NCH - 1)),
                    reads=[sb, k.H.b[cc]], writes=[ps.b[0]], signal=(cc == NCH - 1))
            vi = vcnt % 2
            vcnt += 1
            eng = P.act if vi == 0 else P.dve
            if vi == 0:
                P.op(P.act, lambda e: e.copy(out=VO[:n, vi, :], in_=ps[:n, :]), reads=[ps.b[0]], writes=[VO.b[vi]])
            else:
                P.op(P.dve, lambda e: e.tensor_copy(out=VO[:n, vi, :], in_=ps[:n, :]), reads=[ps.b[0]], writes=[VO.b[vi]])
            P.dma(P.sp, vd[lo:hi, gi * 512:(gi + 1) * 512], VO[:n, vi, :], reads=[VO.b[vi]], dbuf=stv[vi])
        for x_ in sl:
            W.release(x_)
    k.kv_store = stq + stv
    ph.close()


NCHK = 9


def kchunk(kt):
    return kt // 8 if kt < 64 else 8


def load_kv(P, k, KT, VT, k_all, v_all, maps, nk, vcol0, vw, src=()):
    T = TB
    kv = k_all.rearrange("(r m p) t -> p r m t", r=NCORE, m=nk)
    for r in range(NCORE):
        for mi, m in enumerate(maps):
            P.dma(P.sp, KT[:, mi, r * LAT_PC:(r + 1) * LAT_PC], kv[:, r, m, 0:LAT_PC],
                  reads=list(src), writes=[KT.b[r]], dbuf=KT.b[r])
        P.dma(P.sp, VT[:, r * 8:(r + 1) * 8, :],
              v_all[r * T:r * T + LAT_PC, vcol0:vcol0 + vw].rearrange("(j p) n -> p j n", p=128),
              reads=list(src), writes=[VT.b[r]], dbuf=VT.b[r])
    for mi, m in enumerate(maps):
        with P.nc.allow_non_contiguous_dma(reason="ctx keys"):
            P.dma(P.sp, KT[:, mi, KL:KL + CTX].rearrange("p (r t) -> p r t", r=NCORE), kv[:, :, m, LAT_PC:T],
                  reads=list(src), writes=[KT.b[8]], dbuf=KT.b[8])
    for r in range(NCORE):
        P.dma(P.sp, VT[(r % 4) * 32:(r % 4 + 1) * 32, 64 + r // 4, :],
              v_all[r * T + LAT_PC:(r + 1) * T, vcol0:vcol0 + vw],
              reads=list(src), writes=[VT.b[8]], dbuf=VT.b[8])


LOOKAHEAD = 2


SUM_ROLES = ("dve", "pool", "dve", "pe")
SUM_TAIL = 6


def emit_attn_core(P, k, QTm, qcols, KT, mi, VT, vw, PT, pti, kts, ACC, ACCR):
    a, b_ = qcols
    n = b_ - a
    nvo = vw // 128
    npt = len(PT.b)
    pos = [ps_reserve(k) for _ in range(nvo)]
    psm = ps_reserve(k)
    pend = []
    nk_ = len(kts)
    nacc = max(0, nk_ - SUM_TAIL)
    if nacc < 4:
        nacc = 0
    roles = [SUM_ROLES[ii % 4] if ii < nacc else "pe" for ii in range(nk_)]
    first_of = {r: roles.index(r) for r in set(roles)}
    st = {"pe_started": False}

    def flush():
        ii, kt, pi = pend.pop(0)
        first, last = ii == 0, ii == nk_ - 1
        ch = kchunk(kt)
        role = roles[ii]
        for j in range(nvo):
            P.op(P.pe, lambda e, j=j: e.matmul(pos[j][:, :n], VT[:, kt, j * 128:(j + 1) * 128], PT[:, pi, :n], start=first, stop=last),
                 reads=[VT.b[ch], PT.b[pi]], writes=[pos[j].b[0]], signal=(last or (j == nvo - 1 and role != "pe")))
        if role == "pe":
            P.op(P.pe, lambda e: e.matmul(psm[:, :n], k.ONEB[:, :], PT[:, pi, :n], start=not st["pe_started"], stop=(last and nacc == 0)),
                 reads=[k.ONEB.b[0], PT.b[pi]], writes=[psm.b[0]], signal=True)
            st["pe_started"] = True
        else:
            E = P.dve if role == "dve" else P.pool
            ai = 0 if role == "dve" else 1
            acc, accb = ACC[:, ai, :n], ACC.b[ai]
            if ii == first_of[role]:
                P.op(E, lambda e: e.tensor_copy(out=acc, in_=PT[:, pi, :n]), reads=[PT.b[pi]], writes=[accb])
            else:
                P.op(E, lambda e: e.tensor_tensor(out=acc, in0=acc, in1=PT[:, pi, :n], op=ALU.add),
                     reads=[PT.b[pi], accb], writes=[accb])
        if nacc and ii == nacc - 1:
            P.op(P.dve, lambda e: e.tensor_tensor(out=ACCR[:, 0, :n].bitcast(F32R), in0=ACC[:, 0, :n], in1=ACC[:, 1, :n], op=ALU.add),
                 reads=[ACC.b[0], ACC.b[1]], writes=[ACCR.b[0]])

    for ii, kt in enumerate(kts):
        ps = psum(k)
        ch = kchunk(kt)
        P.op(P.pe, lambda e: e.matmul(ps[:, :n], KT[:, mi, kt * 128:(kt + 1) * 128], QTm(a, b_), start=True, stop=True),
             reads=[KT.b[ch], k.QT.b[0]], writes=[ps.b[0]])
        pi = pti[0] % npt
        pti[0] += 1
        P.op(P.act, lambda e: e.activation(out=PT[:, pi, :n], in_=ps[:, :n], func=AF.Exp),
             reads=[ps.b[0]], writes=[PT.b[pi]])
        pend.append((ii, kt, pi))
        if len(pend) > LOOKAHEAD:
            flush()
    while pend:
        flush()
    if nacc:
        P.op(P.pe, lambda e: e.matmul(psm[:, :n], k.ONER[:, :].bitcast(F32R), ACCR[:, 0, :n].bitcast(F32R),
                                      start=not st["pe_started"], stop=True),
             reads=[k.ONER.b[0], ACCR.b[0]], writes=[psm.b[0]], signal=True)
    return pos, psm


def emit_attn_gqa(P, k, q_d, k_all, v_all, src=()):
    ph = Phase(P)
    T = k.T
    KT = ph.tt("KT", [128, 1, KL + CTX], BF16, nbuf=NCHK)
    VT = ph.tt("VT", [128, NKT, 128], BF16, nbuf=NCHK)
    k.QT = ph.tt("QT", [128, 4, T], BF16)
    PT = ph.tt("PT", [128, 4, 512], BF16, nbuf=4)
    REC = ph.tt("REC", [128, 512], F32)
    ACC = ph.tt("ACC", [128, 2, 512], F32, nbuf=2)
    ACCR = ph.tt("ACCR", [128, 1, 512], F32)
    pti = [0]
    for g in range(4):
        load_kv(P, k, KT, VT, k_all, v_all, [g], 4, g * 128, 128, src)
        P.dma(P.sp, k.QT[:, :, :], q_d[g * 512:(g + 1) * 512, :].rearrange("(h p) t -> p h t", p=128),
              writes=[k.QT.b[0]], dbuf=k.QT.b[0])
        for hh in range(4):
            hq = g * 4 + hh
            work = [((0, 512), list(range(NKT))), ((512, 1024), list(range(NKT))), ((1024, T), [64, 65])]
            for (a, b_), kts in work:
                n = b_ - a
                pos, psm = emit_attn_core(P, k, lambda x, y: k.QT[:, hh, x:y], (a, b_), KT, 0, VT, 128, PT, pti, kts, ACC, ACCR)
                P.op(P.dve, lambda e: e.reciprocal(out=REC[:, :n], in_=psm[:, :n]), reads=[psm.b[0]], writes=[REC.b[0]])
                P.op(P.dve, lambda e: e.tensor_tensor(out=k.H[:, hq, a:b_], in0=pos[0][:, :n], in1=REC[:, :n], op=ALU.mult),
                     reads=[pos[0].b[0], REC.b[0]], writes=[k.H.b[hq]])
                for t_ in pos + [psm]:
                    ps_unreserve(k, t_)
    ph.close()


def emit_attn_diff(P, k, q_d, k_all, v_all, src=()):
    ph = Phase(P)
    T = k.T
    KT = ph.tt("KT", [128, 2, KL + CTX], BF16, nbuf=NCHK)
    VT = ph.tt("VT", [128, NKT, 256], BF16, nbuf=NCHK)
    k.QT = ph.tt("QT", [128, 2, LAT_PC], BF16)
    PT = ph.tt("PT", [128, 4, 512], BF16, nbuf=4)
    REC = ph.tt("REC", [128, 512], F32)
    ACC = ph.tt("ACC", [128, 2, 512], F32, nbuf=2)
    ACCR = ph.tt("ACCR", [128, 1, 512], F32)
    O0 = ph.tt("O0", [128, 2, 512], F32)
    OO = ph.tt("OO", [128, 2, 512], F32)
    LAM = ph.tt("LAM", [128, 8], F32)
    ones = k.CST[:, C_ONES:C_ONES + 128]
    lam = pvs(k, "lam")
    rw = dict(reads=[LAM.b[0], k.PV.b[0]], writes=[LAM.b[0]])
    P.op(P.dve, lambda e: e.tensor_tensor(out=LAM[:, 0:1], in0=lam[:, 0:1], in1=lam[:, 1:2], op=ALU.mult), **rw)
    P.op(P.dve, lambda e: e.tensor_tensor(out=LAM[:, 1:2], in0=lam[:, 2:3], in1=lam[:, 3:4], op=ALU.mult), **rw)
    psl = psum(k)
    P.op(P.pe, lambda e: e.matmul(psl[:, 0:2], ones, LAM[:, 0:2], start=True, stop=True),
         reads=[LAM.b[0], k.CST.b[0]], writes=[psl.b[0]])
    P.op(P.act, lambda e: e.activation(out=LAM[:, 2:4], in_=psl[:, 0:2], func=AF.Exp), reads=[psl.b[0]], writes=[LAM.b[0]])
    P.op(P.dve, lambda e: e.tensor_tensor(out=LAM[:, 4:5], in0=LAM[:, 3:4], in1=LAM[:, 2:3], op=ALU.subtract), **rw)
    P.op(P.dve, lambda e: e.tensor_scalar(out=LAM[:, 4:5], in0=LAM[:, 4:5], scalar1=-LAM_INIT3, scalar2=None, op0=ALU.add), **rw)
    P.op(P.dve, lambda e: e.tensor_scalar(out=LAM[:, 5:7], in0=pvs(k, "subln"), scalar1=1.0 - LAM_INIT3, scalar2=None, op0=ALU.mult), **rw)
    nlam = LAM[:, 4:5]
    pti = [0]
    for h in range(8):
        load_kv(P, k, KT, VT, k_all, v_all, [2 * h, 2 * h + 1], 16, h * 256, 256, src)
        P.dma(P.sp, k.QT[:, :, :], q_d[h * 256:(h + 1) * 256, 0:LAT_PC].rearrange("(m p) t -> p m t", p=128),
              writes=[k.QT.b[0]], dbuf=k.QT.b[0])
        for (a, b_) in ((0, 512), (512, 1024)):
            n = b_ - a
            for mp in range(2):
                pos, psm = emit_attn_core(P, k, lambda x, y: k.QT[:, mp, x:y], (a, b_), KT, mp, VT, 256, PT, pti, list(range(NKT)), ACC, ACCR)
                P.op(P.dve, lambda e: e.reciprocal(out=REC[:, :n], in_=psm[:, :n]), reads=[psm.b[0]], writes=[REC.b[0]])
                for j in range(2):
                    if mp == 0:
                        P.op(P.dve, lambda e, j=j: e.tensor_tensor(out=O0[:, j, :n], in0=pos[j][:, :n], in1=REC[:, :n], op=ALU.mult),
                             reads=[pos[j].b[0], REC.b[0]], writes=[O0.b[0]])
                    else:
                        P.op(P.dve, lambda e, j=j: e.tensor_tensor(out=OO[:, j, :n], in0=pos[j][:, :n], in1=REC[:, :n], op=ALU.mult),
                             reads=[pos[j].b[0], REC.b[0]], writes=[OO.b[0]])
                        P.op(P.dve, lambda e, j=j: e.scalar_tensor_tensor(out=OO[:, j, :n], in0=OO[:, j, :n], scalar=nlam, in1=O0[:, j, :n],
                                                                          op0=ALU.mult, op1=ALU.add),
                             reads=[OO.b[0], O0.b[0], LAM.b[0]], writes=[OO.b[0]])
                for t_ in pos + [psm]:
                    ps_unreserve(k, t_)
            pss = psum(k)
            for j in range(2):
                sq, sqb = scr(k)
                P.op(P.act, lambda e, j=j: e.activation(out=sq[:, :n], in_=OO[:, j, :n], func=AF.Square), reads=[OO.b[0]], writes=[sqb])
                P.op(P.pe, lambda e, j=j: e.matmul(pss[:, :n], ones, sq[:, :n], start=(j == 0), stop=(j == 1)),
                     reads=[sqb, k.CST.b[0]], writes=[pss.b[0]])
            sq, sqb = scr(k)
            P.op(P.act, lambda e: e.activation(out=sq[:, :n], in_=pss[:, :n], func=AF.Sqrt, bias=k.EPS[:, 1:2], scale=1.0 / 256),
                 reads=[pss.b[0], k.EPS.b[0]], writes=[sqb])
            P.op(P.dve, lambda e: e.reciprocal(out=sq[:, :n], in_=sq[:, :n]), reads=[sqb], writes=[sqb])
            for j in range(2):
                P.op(P.dve, lambda e, j=j: e.scalar_tensor_tensor(out=k.H[:, 2 * h + j, a:b_], in0=OO[:, j, :n], scalar=LAM[:, 5 + j:6 + j],
                                                                  in1=sq[:, :n], op0=ALU.mult, op1=ALU.mult),
                     reads=[OO.b[0], sqb, LAM.b[0]], writes=[k.H.b[2 * h + j]])
    for c in range(NCH):
        P.op(P.pool, lambda e, c=c: e.memset(k.H[:, c, LAT_PC:T], 0.0), writes=[k.H.b[c]])
    ph.close()


def plan_qkv(layer):
    p = []
    if layer == 2:
        name, nm, vcol, ng = "gqa_qkv_w", 20, 2560, 1
    else:
        name, nm, vcol, ng = "diff_qkv_w", 32, 4096, 4
    for m in range(0, nm, 2):
        p.append(("proj", name, 0, [m * 128, (m + 1) * 128]))
    for gi in range(ng):
        p.append(("vproj", name, 0, 0, vcol + gi * 512))
        p.append(("vproj", name, 0, 8, vcol + gi * 512))
    return p


def plan_outproj(name):
    return [("proj", name, 0, [d * 128, (d + 1) * 128]) for d in range(0, 16, 2)]


def plan_launch(which, stop_after=None):
    if which == "A":
        assert stop_after is None
        return plan_mod(0) + plan_layer0() + plan_moe(0, side=1) + plan_layer1() + plan_moe(1, side=2) + plan_qkv(2)
    if which == "B":
        return [("fence",)] + plan_outproj("gqa_out_w") + plan_moe(2, side=3) + plan_qkv(3)
    if which == "C":
        return [("fence",)] + plan_outproj("diff_out_w") + plan_moe(3)
    raise ValueError(which)


BF = mybir.dt.bfloat16


def build_launch(which, pv, plan, stop_after=None):
    nc = bass.Bass("TRN2", target_bir_lowering=False)
    P = Prog(nc)
    TX = TA if which == "A" else TB
    nslots = len([sp for sp in plan if sp[0] != "fence"])
    xin = nc.dram_tensor("xin", [128, NCH, TX], F32, kind="ExternalInput").ap()
    pvd = nc.dram_tensor("pvec", [128, pv.n], F32, kind="ExternalInput").ap()
    cstd = nc.dram_tensor("cst", [128, 384], F32, kind="ExternalInput").ap()
    wd = nc.dram_tensor("wstream", [nslots, 128, SLOT], F32, kind="ExternalInput").ap()
    xout = nc.dram_tensor("xout", [128, NCH, TB], F32, kind="ExternalOutput").ap()
    layers = {"A": [0, 1, 2], "B": [2, 3], "C": [3]}[which]
    full = stop_after is None
    if which in ("A", "B") and full:
        nk_o = 4 if which == "A" else 16
        nv_o = 512 if which == "A" else 2048
        ropec = nc.dram_tensor("ropec", [128, LAT_PC], F32, kind="ExternalInput").ap()
        ropes = nc.dram_tensor("ropes", [128, LAT_PC], F32, kind="ExternalInput").ap()
        qd_o = nc.dram_tensor("qd_o", [16 * 128, TB], BF, kind="ExternalOutput").ap()
        kd_o = nc.dram_tensor("kd_o", [nk_o * 128, TB], BF, kind="ExternalOutput").ap()
        vd_o = nc.dram_tensor("vd_o", [TB, nv_o], BF, kind="ExternalOutput").ap()
    if which in ("B", "C"):
        nk_i = 4 if which == "B" else 16
        nv_i = 512 if which == "B" else 2048
        qd_i = nc.dram_tensor("qd_i", [16 * 128, TB], BF, kind="ExternalInput").ap()
        k_all = nc.dram_tensor("k_all", [NCORE * nk_i * 128, TB], BF, kind="ExternalInput").ap()
        v_all = nc.dram_tensor("v_all", [NCORE * TB, nv_i], BF, kind="ExternalInput").ap()
    k = K()
    setup_common(P, k, TX, NLA if which == "A" else LAT_PC, pv, layers, pvd, cstd)
    ld = Buf("ldx")
    if which == "A":
        vmd = nc.dram_tensor("vm", [128, TA], F32, kind="ExternalInput").ap()
        k.VM = TT(P, "VM", [128, TA], F32)
        P.dma(P.sp, k.VM[:, :], vmd, writes=[k.VM.b[0]], dbuf=ld)
    for c4 in range(0, NCH, 4):
        P.dma(P.sp, k.X[:, c4:c4 + 4, :], xin[:, c4:c4 + 4, :], writes=k.X.b[c4:c4 + 4], dbuf=ld)
    P.fix(([k.VM.b[0]] if which == "A" else []) + k.X.b, ld)
    W = WStream(P, plan, wd)
    emit_silu_c(P, k)
    if which == "A":
        emit_mod(P, k, W, 0)
    else:
        modst_i = nc.dram_tensor("modst_i", [128, 384], F32, kind="ExternalInput").ap()
        li0 = k.lidx[layers[0]]
        P.dma(P.sp, k.MOD[:, li0, :, :], modst_i[:, 0:192].rearrange("p (a b) -> p a b", a=2),
              writes=[k.MOD.b[0]], dbuf=ld)
        P.dma(P.sp, k.DER[:, li0, :, :, :], modst_i[:, 192:384].rearrange("p (a b c) -> p a b c", a=2, b=6),
              writes=[k.DER.b[0]], dbuf=ld)
        P.fix([k.MOD.b[0], k.DER.b[0]] + k.X.b, ld)
    if which in ("A", "B"):
        modst_o = nc.dram_tensor("modst_o", [128, 384], F32, kind="ExternalOutput").ap()

    def tail(layer, side=None):
        emit_ln(P, k, layer, 0, True)
        emit_moe(P, k, W, layer, side=side)
        emit_ln(P, k, layer, 1, False)

    if which == "A":
        emit_modulate(P, k, 0)
        emit_conv_mixer(P, k, W, 0)
        tail(0, side=mod_steps(P, k, W, 1))
        emit_modulate(P, k, 1)
        emit_pool_mixer(P, k, W, 1)
        tail(1, side=mod_steps(P, k, W, 2))
        ph = Phase(P)
        TMP = ph.tt("TMP", [128, 2, LAT_PC], F32, nbuf=2)
        for c in range(NCH):
            z = c % 2
            e1, e2 = (P.dve, P.pool) if z == 0 else (P.pool, P.dve)
            P.op(e1, lambda e, c=c, z=z: e.tensor_copy(out=TMP[:, z, :], in_=k.X[:, c, HALO:HALO + LAT_PC]),
                 reads=[k.X.b[c]], writes=[TMP.b[z]])
            P.op(P.act, lambda e, c=c: e.copy(out=k.X[:, c, LAT_PC:TB], in_=k.X[:, c, NLA + HALO:NLA + HALO + CTX_PC]),
                 reads=[k.X.b[c]], writes=[k.X.b[c]])
            P.op(e2, lambda e, c=c, z=z: e.tensor_copy(out=k.X[:, c, 0:LAT_PC], in_=TMP[:, z, :]),
                 reads=[TMP.b[z], k.X.b[c]], writes=[k.X.b[c]])
        ph.close()
        k.T, k.nl = TB, LAT_PC
        k.tiles = tok_tiles(TB)
        if full:
            emit_modulate(P, k, 2)
            emit_qkv(P, k, W, 2, qd_o, kd_o, vd_o, ropec, ropes)
    elif which == "B":
        W.suspend()
        emit_attn_gqa(P, k, qd_i, k_all, v_all)
        W.resume()
        li = k.lidx[2]
        emit_outproj(P, k, W, k.H, lambda cls, d: k.DER[:, li, cls, 2, d:d + 1])
        tail(2, side=mod_steps(P, k, W, 3))
        emit_modulate(P, k, 3)
        emit_qkv(P, k, W, 3, qd_o, kd_o, vd_o, ropec, ropes)
    else:
        W.suspend()
        emit_attn_diff(P, k, qd_i, k_all, v_all)
        W.resume()
        li = k.lidx[3]
        emit_outproj(P, k, W, k.H, lambda cls, d: k.DER[:, li, cls, 2, d:d + 1])
        tail(3)
    st = Buf("st")
    for c4 in range(0, NCH, 4):
        P.dma(P.sp, xout[:, c4:c4 + 4, :], k.X[:, c4:c4 + 4, 0:TB], reads=k.X.b[c4:c4 + 4], dbuf=st)
    if which in ("A", "B"):
        lo_ = k.lidx[layers[-1]]
        P.dma(P.sp, modst_o[:, 0:192].rearrange("p (a b) -> p a b", a=2), k.MOD[:, lo_, :, :],
              reads=[k.MOD.b[0]], dbuf=st)
        P.dma(P.sp, modst_o[:, 192:384].rearrange("p (a b c) -> p a b c", a=2, b=6), k.DER[:, lo_, :, :, :],
              reads=[k.DER.b[0]], dbuf=st)
    P.sp.e.wait_ge(st.dsem, st.dcnt)
    if hasattr(k, "kv_store"):
        for b_ in k.kv_store:
            if b_.dsem is not None:
                P.sp.e.wait_ge(b_.dsem, b_.dcnt)
    assert P.pe.last_signaled
    assert W.cur == len(plan), (W.cur, len(plan))
    return nc, P


def plan_fused():
    return (plan_mod(0) + plan_layer0() + plan_moe(0, side=1) + plan_layer1() + plan_moe(1, side=2)
            + plan_qkv(2) + [("fence",)] + plan_outproj("gqa_out_w") + plan_moe(2, side=3)
            + plan_qkv(3) + [("fence",)] + plan_outproj("diff_out_w") + plan_moe(3))


def emit_gather(P, src, dst, name):
    b = Buf(name)
    b.dsem = P.nc.alloc_semaphore("csem_" + name)
    P.sems.append(b.dsem)
    ins = P.pool.e.collective_compute("AllGather", ALU.bypass, replica_groups=[list(range(NCORE))], ins=[src], outs=[dst])
    ins.then_inc(b.dsem, 16)
    b.dcnt = 16
    b.w = (b.dsem, 16)
    return b


def build_fused(pv, plan):
    nc = bass.Bass("TRN2", target_bir_lowering=False)
    P = Prog(nc)
    nslots = len([sp for sp in plan if sp[0] != "fence"])
    xin = nc.dram_tensor("xin", [128, NCH, TA], F32, kind="ExternalInput").ap()
    vmd = nc.dram_tensor("vm", [128, TA], F32, kind="ExternalInput").ap()
    pvd = nc.dram_tensor("pvec", [128, pv.n], F32, kind="ExternalInput").ap()
    cstd = nc.dram_tensor("cst", [128, 384], F32, kind="ExternalInput").ap()
    wd = nc.dram_tensor("wstream", [nslots, 128, SLOT], F32, kind="ExternalInput").ap()
    ropec = nc.dram_tensor("ropec", [128, LAT_PC], F32, kind="ExternalInput").ap()
    ropes = nc.dram_tensor("ropes", [128, LAT_PC], F32, kind="ExternalInput").ap()
    xout = nc.dram_tensor("xout", [128, NCH, LAT_PC], F32, kind="ExternalOutput").ap()
    dr = {}
    for l, nk_, nv_ in ((2, 4, 512), (3, 16, 2048)):
        dr[l] = dict(
            q=nc.dram_tensor("qd%d" % l, [16 * 128, TB], BF, kind="Internal").ap(),
            k=nc.dram_tensor("kd%d" % l, [nk_ * 128, TB], BF, kind="Internal").ap(),
            v=nc.dram_tensor("vd%d" % l, [TB, nv_], BF, kind="Internal").ap(),
            ka=nc.dram_tensor("kall%d" % l, [NCORE * nk_ * 128, TB], BF, kind="Internal").ap(),
            va=nc.dram_tensor("vall%d" % l, [NCORE * TB, nv_], BF, kind="Internal").ap())
    layers = [0, 1, 2, 3]
    k = K()
    setup_common(P, k, TA, NLA, pv, layers, pvd, cstd)
    ld = Buf("ldx")
    k.VM = TT(P, "VM", [128, TA], F32)
    P.dma(P.sp, k.VM[:, :], vmd, writes=[k.VM.b[0]], dbuf=ld)
    for c4 in range(0, NCH, 4):
        P.dma(P.sp, k.X[:, c4:c4 + 4, :], xin[:, c4:c4 + 4, :], writes=k.X.b[c4:c4 + 4], dbuf=ld)
    P.fix([k.VM.b[0]] + k.X.b, ld)
    W = WStream(P, plan, wd)
    emit_silu_c(P, k)
    emit_mod(P, k, W, 0)
    emit_modulate(P, k, 0)
    emit_conv_mixer(P, k, W, 0)
    emit_ln(P, k, 0, 0, True)
    emit_moe(P, k, W, 0, side=mod_steps(P, k, W, 1))
    emit_ln(P, k, 0, 1, False)
    emit_modulate(P, k, 1)
    emit_pool_mixer(P, k, W, 1)
    emit_ln(P, k, 1, 0, True)
    emit_moe(P, k, W, 1, side=mod_steps(P, k, W, 2))
    emit_ln(P, k, 1, 1, False)
    ph = Phase(P)
    TMP = ph.tt("TMP", [128, 2, LAT_PC], F32, nbuf=2)
    for c in range(NCH):
        z = c % 2
        e1, e2 = (P.dve, P.pool) if z == 0 else (P.pool, P.dve)
        P.op(e1, lambda e, c=c, z=z: e.tensor_copy(out=TMP[:, z, :], in_=k.X[:, c, HALO:HALO + LAT_PC]),
             reads=[k.X.b[c]], writes=[TMP.b[z]])
        P.op(P.act, lambda e, c=c: e.copy(out=k.X[:, c, LAT_PC:TB], in_=k.X[:, c, NLA + HALO:NLA + HALO + CTX_PC]),
             reads=[k.X.b[c]], writes=[k.X.b[c]])
        P.op(e2, lambda e, c=c, z=z: e.tensor_copy(out=k.X[:, c, 0:LAT_PC], in_=TMP[:, z, :]),
             reads=[TMP.b[z], k.X.b[c]], writes=[k.X.b[c]])
    ph.close()
    k.T, k.nl = TB, LAT_PC
    k.tiles = tok_tiles(TB)
    for l in (2, 3):
        d = dr[l]
        emit_modulate(P, k, l)
        emit_qkv(P, k, W, l, d["q"], d["k"], d["v"], ropec, ropes)
        gk = emit_gather(P, d["k"], d["ka"], "k%d" % l)
        gv = emit_gather(P, d["v"], d["va"], "v%d" % l)
        W.suspend()
        if l == 2:
            emit_attn_gqa(P, k, d["q"], d["ka"], d["va"], src=[gk, gv])
        else:
            emit_attn_diff(P, k, d["q"], d["ka"], d["va"], src=[gk, gv])
        W.resume()
        li = k.lidx[l]
        emit_outproj(P, k, W, k.H, lambda cls, dd, li=li: k.DER[:, li, cls, 2, dd:dd + 1])
        emit_ln(P, k, l, 0, True)
        emit_moe(P, k, W, l, side=(mod_steps(P, k, W, 3) if l == 2 else None))
        emit_ln(P, k, l, 1, False)
    st = Buf("st")
    for c4 in range(0, NCH, 4):
        P.dma(P.sp, xout[:, c4:c4 + 4, :], k.X[:, c4:c4 + 4, 0:LAT_PC], reads=k.X.b[c4:c4 + 4], dbuf=st)
    P.sp.e.wait_ge(st.dsem, st.dcnt)
    assert P.pe.last_signaled
    assert W.cur == len(plan), (W.cur, len(plan))
    return nc, P


def run_fused(inp, trace=False):
    plan = plan_fused()
    pv = make_pvec(inp, [0, 1, 2, 3])
    ws = stream_array(inp, plan)
    nc, P = build_fused(pv, plan)
    print("fused: instructions", P.ninst, "waits", P.nwait, "slots", ws.shape[0], flush=True)
    xs, vms = prep_A(inp)
    cst = make_consts()
    pva = pv.array()
    in_maps = []
    for r in range(NCORE):
        c_, s_ = rope_tables(r)
        in_maps.append({"xin": xs[r], "vm": vms[r], "pvec": pva, "cst": cst, "wstream": ws, "ropec": c_, "ropes": s_})
    res = run_bass_kernel_spmd(nc, in_maps, core_ids=list(range(NCORE)), trace=trace)
    if trace:
        print("exec_time_ns", res.exec_time_ns, flush=True)
    out = np.empty((1, SEQ, D), np.float32)
    for r in range(NCORE):
        o = res.results[r]["xout"]
        out[0, r * LAT_PC:(r + 1) * LAT_PC] = o.transpose(2, 1, 0).reshape(LAT_PC, D)
    return out


def prep_A(inp):
    x = inp["x"][0]
    ctx = inp["ctx"][0]
    xs, vms = [], []
    for r in range(NCORE):
        buf = np.zeros((TA, D), np.float32)
        vm = np.zeros((TA,), np.float32)
        lo = r * LAT_PC - HALO
        a, b = max(lo, 0), min(lo + NLA, SEQ)
        buf[a - lo:b - lo] = x[a:b]
        vm[a - lo:b - lo] = 1.0
        lo = r * CTX_PC - HALO
        a, b = max(lo, 0), min(lo + NCA, CTX)
        buf[NLA + a - lo:NLA + b - lo] = ctx[a:b]
        vm[NLA + a - lo:NLA + b - lo] = 1.0
        xs.append(np.ascontiguousarray(buf.reshape(TA, NCH, 128).transpose(2, 1, 0)))
        vms.append(np.ascontiguousarray(np.broadcast_to(vm[None, :], (128, TA))))
    return xs, vms


def rope_tables(r):
    pos = np.arange(r * LAT_PC, (r + 1) * LAT_PC)
    row = (pos // GRID_W).astype(np.float32)
    col = (pos % GRID_W).astype(np.float32)
    inv = (10000.0 ** (-np.arange(0, 64, 2, dtype=np.float32) / 64.0)).astype(np.float32)
    ang = np.concatenate([row[None, :] * inv[:, None], row[None, :] * inv[:, None],
                          col[None, :] * inv[:, None], col[None, :] * inv[:, None]], axis=0).astype(np.float32)
    return np.ascontiguousarray(np.cos(ang).astype(np.float32)), np.ascontiguousarray(np.sin(ang).astype(np.float32))


def run_launch(which, inp, state, stop_after=None, trace=False):
    plan = plan_launch(which, stop_after)
    layers = {"A": [0, 1, 2], "B": [2, 3], "C": [3]}[which]
    pv = make_pvec(inp, layers)
    ws = stream_array(inp, plan)
    nc, P = build_launch(which, pv, plan, stop_after)
    print("launch", which, "instructions", P.ninst, "waits", P.nwait, "slots", ws.shape[0], flush=True)
    cst = make_consts()
    pva = pv.array()
    in_maps = []
    for r in range(NCORE):
        m = {"xin": state["x"][r], "pvec": pva, "cst": cst, "wstream": ws}
        if which == "A":
            m["vm"] = state["vm"][r]
        if which in ("A", "B") and stop_after is None:
            c_, s_ = rope_tables(r)
            m["ropec"], m["ropes"] = c_, s_
        if which in ("B", "C"):
            m["modst_i"] = state["modst"][r]
            m["qd_i"] = state["q"][r]
            m["k_all"] = state["k_all"]
            m["v_all"] = state["v_all"]
        in_maps.append(m)
    res = run_bass_kernel_spmd(nc, in_maps, core_ids=list(range(NCORE)), trace=trace)
    if trace:
        print("exec_time_ns", res.exec_time_ns, flush=True)
    out = {"x": [res.results[r]["xout"] for r in range(NCORE)]}
    if which in ("A", "B"):
        out["modst"] = [res.results[r]["modst_o"] for r in range(NCORE)]
    if which in ("A", "B") and stop_after is None:
        out["q"] = [res.results[r]["qd_o"] for r in range(NCORE)]
        out["k_all"] = np.concatenate([res.results[r]["kd_o"] for r in range(NCORE)], axis=0)
        out["v_all"] = np.concatenate([res.results[r]["vd_o"] for r in range(NCORE)], axis=0)
    return out


def kernel(**inp):
    inp = {k_: np.asarray(v) for k_, v in inp.items()}
    xs, vms = prep_A(inp)
    st = run_launch("A", inp, {"x": xs, "vm": vms})
    st = run_launch("B", inp, st)
    st = run_launch("C", inp, st)
    out = np.empty((1, SEQ, D), np.float32)
    for r in range(NCORE):
        o = st["x"][r]
        out[0, r * LAT_PC:(r + 1) * LAT_PC] = o[:, :, 0:LAT_PC].transpose(2, 1, 0).reshape(LAT_PC, D)
    return out
```
